# Optimizing a Trainium2 kernel written in Bass

```python
import jax
import jax.numpy as jnp
from jax import lax
import numpy as np

D_MODEL = 2048
BATCH = 2
SEQ = 4096
DEPTH = 1

GRID_W = 64
CTX_LEN = 256
HEAD_DIM = 64
MIX_DIM = D_MODEL
RWKV_DIM = MIX_DIM // 2
RWKV_HEADS = RWKV_DIM // HEAD_DIM
ATTN_DIM = MIX_DIM - RWKV_DIM
ATTN_HEADS = ATTN_DIM // HEAD_DIM
ATTN_KV_HEADS = 4
ATTN_GROUP = ATTN_HEADS // ATTN_KV_HEADS
KV_DIM = ATTN_KV_HEADS * HEAD_DIM
W_LORA = 96
A_LORA = 96
G_LORA = 256
RWKV_SPLITS = (RWKV_DIM, 2 * RWKV_DIM, 3 * RWKV_DIM, 3 * RWKV_DIM + W_LORA, 3 * RWKV_DIM + W_LORA + A_LORA)
RWKV_COLS = 3 * RWKV_DIM + W_LORA + A_LORA + G_LORA
IN_SPLITS = (RWKV_COLS, RWKV_COLS + ATTN_DIM, RWKV_COLS + ATTN_DIM + KV_DIM)
IN_COLS = RWKV_COLS + ATTN_DIM + 2 * KV_DIM
WINDOW = 128
BLOCK = 128
ROPE_BASE = 10000.0
N_EXPERTS = 64
N_GROUPS = 8
TOPK_GROUPS = 4
TOP_K = 8
EXPERT_FF = 512
SHARED_FF = 512
ROUTED_SCALE = 2.5
EXPERT_BLOCK = 128
NORM_EPS = 1e-6
LNX_EPS = 64e-5

kernel_name = 'hybrid_rwkv7_swa_moe_dit_block'


def rmsnorm(x, g):
    x32 = x.astype(jnp.float32)
    y = x32 * lax.rsqrt(jnp.mean(x32 * x32, axis=-1, keepdims=True) + NORM_EPS)
    return (y * g.astype(jnp.float32)).astype(x.dtype)


def modulate(h, shift, scale):
    return h * (1.0 + scale) + shift


def heads(t, n):
    return t.reshape(t.shape[:-1] + (n, t.shape[-1] // n))


def centred_shift(p, mu):
    prev = jnp.pad(p[:, :-1], ((0, 0), (1, 0), (0, 0)))
    nxt = jnp.pad(p[:, 1:], ((0, 0), (0, 1), (0, 0)))
    return p + mu * (0.5 * (prev + nxt) - p)


def rope_pairs(x, ang):
    x1, x2 = jnp.split(x, 2, axis=-1)
    cos = jnp.cos(ang)[None, :, None, :].astype(x.dtype)
    sin = jnp.sin(ang)[None, :, None, :].astype(x.dtype)
    return jnp.concatenate([x1 * cos - x2 * sin, x2 * cos + x1 * sin], axis=-1)


def axial_rope(x, row, col):
    n_freq = HEAD_DIM // 4
    freqs = ROPE_BASE ** (-jnp.arange(n_freq, dtype=jnp.float32) / n_freq)
    xr, xc = jnp.split(x, 2, axis=-1)
    return jnp.concatenate([rope_pairs(xr, row[:, None] * freqs), rope_pairs(xc, col[:, None] * freqs)], axis=-1)


def wkv7_scan(s0, r, w, k, v, a_, b_, reverse, emit):
    def step(s, inp):
        r_t, w_t, k_t, v_t, a_t, b_t = inp
        sa = jnp.einsum('bhvk,bhk->bhv', s, a_t)
        s = s * w_t[:, :, None, :] + sa[..., None] * b_t[:, :, None, :] + v_t[..., None] * k_t[:, :, None, :]
        y = jnp.einsum('bhvk,bhk->bhv', s, r_t) if emit else None
        return s, y
    xs = tuple(jnp.swapaxes(t, 0, 1) for t in (r, w, k, v, a_, b_))
    s, ys = lax.scan(step, s0, xs, reverse=reverse)
    return s, (jnp.swapaxes(ys, 0, 1) if emit else None)


def rwkv_streams(p, shift_mu, decay_bias, decay_up, iclr_bias, iclr_up, k_k, k_a):
    p = centred_shift(p, shift_mu)
    r, k, v, xw, xa, xg = jnp.split(p, RWKV_SPLITS, axis=-1)
    kk = heads((k * k_k).astype(jnp.float32), RWKV_HEADS)
    kk = kk * lax.rsqrt(jnp.sum(kk * kk, axis=-1, keepdims=True) + 1e-12)
    tw = jnp.tanh(xw)
    dirs = []
    for d in range(2):
        logw = -jax.nn.softplus(-(decay_bias[d] + tw @ decay_up[d])) - 0.5
        decay = jnp.exp(-jnp.exp(logw.astype(jnp.float32)))
        a = jax.nn.sigmoid((iclr_bias[d] + xa @ iclr_up[d]).astype(jnp.float32))
        k_d = k.astype(jnp.float32) * (1.0 + (a - 1.0) * k_a.astype(jnp.float32))
        dirs.append((heads(decay, RWKV_HEADS), heads(k_d, RWKV_HEADS), kk * heads(a, RWKV_HEADS)))
    r = heads(r.astype(jnp.float32), RWKV_HEADS)
    v = heads(v.astype(jnp.float32), RWKV_HEADS)
    return r, v, xg, -kk, dirs


def rwkv_output(y, r, v, k_sum, xg, gate_up, r_k, lnx_g, lnx_b):
    b, t = y.shape[:2]
    mean = jnp.mean(y, axis=-1, keepdims=True)
    var = jnp.mean((y - mean) ** 2, axis=-1, keepdims=True)
    yn = ((y - mean) * lax.rsqrt(var + LNX_EPS)).reshape(b, t, RWKV_DIM)
    bonus = jnp.sum(r * k_sum * r_k.astype(jnp.float32), axis=-1, keepdims=True) * v
    out = yn * lnx_g.astype(jnp.float32) + lnx_b.astype(jnp.float32) + bonus.reshape(b, t, RWKV_DIM)
    g = jax.nn.sigmoid(xg) @ gate_up
    return out.astype(xg.dtype) * g


def window_ctx_attention(q, k, v, k_ctx, v_ctx, sink):
    b, t = q.shape[:2]
    nb = t // BLOCK
    qb = q.reshape(b, nb, BLOCK, ATTN_KV_HEADS, ATTN_GROUP, HEAD_DIM)

    def band(z):
        zp = jnp.pad(z, ((0, 0), (BLOCK, BLOCK), (0, 0), (0, 0))).reshape(b, nb + 2, BLOCK, ATTN_KV_HEADS, HEAD_DIM)
        return jnp.concatenate([zp[:, :-2], zp[:, 1:-1], zp[:, 2:]], axis=2)

    kw, vw = band(k), band(v)
    qpos = jnp.arange(t).reshape(nb, BLOCK)
    kpos = (jnp.arange(nb)[:, None] - 1) * BLOCK + jnp.arange(3 * BLOCK)[None, :]
    valid = (jnp.abs(qpos[:, :, None] - kpos[:, None, :]) <= WINDOW) & (kpos[:, None, :] >= 0) & (kpos[:, None, :] < t)
    scale = HEAD_DIM ** -0.5
    s_w = jnp.einsum('bnqkgd,bnskd->bnkgqs', qb, kw).astype(jnp.float32) * scale
    s_w = jnp.where(valid[None, :, None, None], s_w, -jnp.inf)
    s_c = jnp.einsum('bnqkgd,bckd->bnkgqc', qb, k_ctx).astype(jnp.float32) * scale
    sk = sink.reshape(ATTN_KV_HEADS, ATTN_GROUP)[None, None, :, :, None, None].astype(jnp.float32)
    m = jnp.maximum(jnp.maximum(jnp.max(s_w, -1, keepdims=True), jnp.max(s_c, -1, keepdims=True)), sk)
    e_w = jnp.exp(s_w - m)
    e_c = jnp.exp(s_c - m)
    denom = jnp.sum(e_w, -1, keepdims=True) + jnp.sum(e_c, -1, keepdims=True) + jnp.exp(sk - m)
    o = (jnp.einsum('bnkgqs,bnskd->bnqkgd', (e_w / denom).astype(v.dtype), vw)
         + jnp.einsum('bnkgqc,bckd->bnqkgd', (e_c / denom).astype(v.dtype), v_ctx))
    return o.reshape(b, t, ATTN_DIM)


def ctx_attention(q, k, v, sink):
    b, c = q.shape[:2]
    qg = q.reshape(b, c, ATTN_KV_HEADS, ATTN_GROUP, HEAD_DIM)
    s = jnp.einsum('bqkgd,bckd->bkgqc', qg, k).astype(jnp.float32) * HEAD_DIM ** -0.5
    sk = sink.reshape(ATTN_KV_HEADS, ATTN_GROUP)[None, :, :, None, None].astype(jnp.float32)
    m = jnp.maximum(jnp.max(s, -1, keepdims=True), sk)
    e = jnp.exp(s - m)
    p = e / (jnp.sum(e, -1, keepdims=True) + jnp.exp(sk - m))
    return jnp.einsum('bkgqc,bckd->bqkgd', p.astype(v.dtype), v).reshape(b, c, ATTN_DIM)


def token_mixers(px, pc, row, col, update_ctx, shift_mu, k_k, k_a, r_k, decay_bias, decay_up,
                 iclr_bias, iclr_up, gate_up, lnx_g, lnx_b, attn_sink):
    rx, qx, kx, vx = jnp.split(px, IN_SPLITS, axis=-1)
    rc, qc, kc, vc = jnp.split(pc, IN_SPLITS, axis=-1)
    lora = (shift_mu, decay_bias, decay_up, iclr_bias, iclr_up, k_k, k_a)
    r_x, v_x, xg_x, na_x, dirs_x = rwkv_streams(rx, *lora)
    r_c, v_c, xg_c, na_c, dirs_c = rwkv_streams(rc, *lora)
    s0 = jnp.zeros((px.shape[0], RWKV_HEADS, HEAD_DIM, HEAD_DIM), jnp.float32)
    ys_x, ys_c = [], []
    for d, reverse in enumerate((False, True)):
        dec_c, k_c, b_c = dirs_c[d]
        s_ctx, y_c = wkv7_scan(s0, r_c, dec_c, k_c, v_c, na_c, b_c, reverse, update_ctx)
        dec_x, k_x, b_x = dirs_x[d]
        _, y_x = wkv7_scan(s_ctx, r_x, dec_x, k_x, v_x, na_x, b_x, reverse, True)
        ys_x.append(y_x)
        ys_c.append(y_c)
    rwkv_x = rwkv_output(ys_x[0] + ys_x[1], r_x, v_x, dirs_x[0][1] + dirs_x[1][1], xg_x, gate_up, r_k, lnx_g, lnx_b)
    q = axial_rope(heads(qx, ATTN_HEADS), row, col)
    k = axial_rope(heads(kx, ATTN_KV_HEADS), row, col)
    k_ctx = heads(kc, ATTN_KV_HEADS)
    v_ctx = heads(vc, ATTN_KV_HEADS)
    attn_x = window_ctx_attention(q, k, heads(vx, ATTN_KV_HEADS), k_ctx, v_ctx, attn_sink)
    mix_x = jnp.concatenate([rwkv_x, attn_x], axis=-1)
    if not update_ctx:
        return mix_x, None
    rwkv_c = rwkv_output(ys_c[0] + ys_c[1], r_c, v_c, dirs_c[0][1] + dirs_c[1][1], xg_c, gate_up, r_k, lnx_g, lnx_b)
    attn_c = ctx_attention(heads(qc, ATTN_HEADS), k_ctx, v_ctx, attn_sink)
    return mix_x, jnp.concatenate([rwkv_c, attn_c], axis=-1)


def swiglu(h, wg, wu, wd):
    return (jax.nn.silu(h @ wg) * (h @ wu)) @ wd


def grouped_experts(h, idx, gate, e_gate, e_up, e_down):
    n_tok, d = h.shape
    m = n_tok * TOP_K
    n_blk = -(-m // EXPERT_BLOCK) + N_EXPERTS
    e = idx.reshape(m)
    tok = jnp.repeat(jnp.arange(n_tok, dtype=jnp.int32), TOP_K)
    wt = gate.reshape(m)
    order = jnp.argsort(e)
    e_s, tok_s, wt_s = e[order], tok[order], wt[order]
    counts = jnp.bincount(e, length=N_EXPERTS)
    padded = (counts + EXPERT_BLOCK - 1) // EXPERT_BLOCK * EXPERT_BLOCK
    start = jnp.cumsum(counts) - counts
    pend = jnp.cumsum(padded)
    pstart = pend - padded
    dest = pstart[e_s] + (jnp.arange(m) - start[e_s])
    pad_tok = jnp.full((n_blk * EXPERT_BLOCK,), n_tok, jnp.int32).at[dest].set(tok_s)
    pad_wt = jnp.zeros((n_blk * EXPERT_BLOCK,), h.dtype).at[dest].set(wt_s)
    blk_expert = jnp.minimum(jnp.searchsorted(pend, jnp.arange(n_blk) * EXPERT_BLOCK, side='right'), N_EXPERTS - 1)
    h_pad = jnp.concatenate([h, jnp.zeros((1, d), h.dtype)], axis=0)

    def body(y, blk):
        tok_b, wt_b, e_b = blk
        xb = h_pad[tok_b]
        out = swiglu(xb, e_gate[e_b], e_up[e_b], e_down[e_b])
        return y.at[tok_b].add(out * wt_b[:, None]), None

    y, _ = lax.scan(body, jnp.zeros((n_tok + 1, d), h.dtype),
                    (pad_tok.reshape(n_blk, EXPERT_BLOCK), pad_wt.reshape(n_blk, EXPERT_BLOCK), blk_expert))
    return y[:n_tok]


def moe_ffn(h, w_router, expert_bias, e_gate, e_up, e_down, s_gate, s_up, s_down):
    n_tok = h.shape[0]
    scores = jax.nn.sigmoid((h @ w_router).astype(jnp.float32))
    biased = scores + expert_bias.astype(jnp.float32)
    grp = biased.reshape(n_tok, N_GROUPS, N_EXPERTS // N_GROUPS)
    grp_score = jnp.sum(lax.top_k(grp, 2)[0], axis=-1)
    _, top_grp = lax.top_k(grp_score, TOPK_GROUPS)
    grp_mask = jnp.any(top_grp[..., None] == jnp.arange(N_GROUPS), axis=1)
    masked = jnp.where(jnp.repeat(grp_mask, N_EXPERTS // N_GROUPS, axis=1), biased, -jnp.inf)
    _, idx = lax.top_k(masked, TOP_K)
    gate = jnp.take_along_axis(scores, idx, axis=1)
    gate = gate / jnp.sum(gate, axis=-1, keepdims=True) * ROUTED_SCALE
    routed = grouped_experts(h, idx, gate.astype(h.dtype), e_gate, e_up, e_down)
    return routed + swiglu(h, s_gate, s_up, s_down)


def setup_inputs(seed: int = 0) -> dict:
    key = jax.random.key(seed)
    ks = jax.random.split(key, 32)
    f32 = jnp.float32

    def nrm(k, shape, scale):
        return jax.random.normal(k, shape, f32) * scale

    d = D_MODEL
    return {
        'x': nrm(ks[0], (BATCH, SEQ, d), 1.0),
        'c': nrm(ks[1], (BATCH, d), 1.0),
        'ctx': nrm(ks[2], (BATCH, CTX_LEN, d), 1.0),
        'c_ctx': nrm(ks[3], (d,), 1.0),
        'w_mod': nrm(ks[4], (DEPTH, d, 6 * d), 0.5 * d ** -0.5),
        'b_mod': nrm(ks[5], (DEPTH, 6 * d), 0.02),
        'norm1_g': 1.0 + nrm(ks[6], (DEPTH, d), 0.02),
        'norm2_g': 1.0 + nrm(ks[7], (DEPTH, d), 0.02),
        'w_in': nrm(ks[8], (DEPTH, d, IN_COLS), d ** -0.5),
        'shift_mu': jax.random.uniform(ks[9], (DEPTH, RWKV_COLS), f32),
        'k_k': 0.85 + nrm(ks[10], (DEPTH, RWKV_DIM), 0.02),
        'k_a': 1.0 + nrm(ks[11], (DEPTH, RWKV_DIM), 0.02),
        'r_k': nrm(ks[12], (DEPTH, RWKV_HEADS, HEAD_DIM), 0.1),
        'decay_bias': -3.0 + nrm(ks[13], (DEPTH, 2, RWKV_DIM), 1.0),
        'decay_up': nrm(ks[14], (DEPTH, 2, W_LORA, RWKV_DIM), W_LORA ** -0.5),
        'iclr_bias': nrm(ks[15], (DEPTH, 2, RWKV_DIM), 0.5),
        'iclr_up': nrm(ks[16], (DEPTH, 2, A_LORA, RWKV_DIM), A_LORA ** -0.5),
        'gate_up': nrm(ks[17], (DEPTH, G_LORA, RWKV_DIM), G_LORA ** -0.5),
        'lnx_g': 1.0 + nrm(ks[18], (DEPTH, RWKV_DIM), 0.02),
        'lnx_b': nrm(ks[19], (DEPTH, RWKV_DIM), 0.02),
        'attn_sink': nrm(ks[20], (DEPTH, ATTN_HEADS), 1.0),
        'w_out': nrm(ks[21], (DEPTH, MIX_DIM, d), MIX_DIM ** -0.5),
        'w_router': nrm(ks[22], (DEPTH, d, N_EXPERTS), d ** -0.5),
        'expert_bias': nrm(ks[23], (DEPTH, N_EXPERTS), 0.01),
        'e_gate': nrm(ks[24], (DEPTH, N_EXPERTS, d, EXPERT_FF), d ** -0.5),
        'e_up': nrm(ks[25], (DEPTH, N_EXPERTS, d, EXPERT_FF), d ** -0.5),
        'e_down': nrm(ks[26], (DEPTH, N_EXPERTS, EXPERT_FF, d), EXPERT_FF ** -0.5),
        's_gate': nrm(ks[27], (DEPTH, d, SHARED_FF), d ** -0.5),
        's_up': nrm(ks[28], (DEPTH, d, SHARED_FF), d ** -0.5),
        's_down': nrm(ks[29], (DEPTH, SHARED_FF, d), SHARED_FF ** -0.5),
        'final_g': 1.0 + nrm(ks[30], (d,), 0.02),
    }


def reference(x, c, ctx, c_ctx, w_mod, b_mod, norm1_g, norm2_g, w_in, shift_mu, k_k, k_a, r_k,
              decay_bias, decay_up, iclr_bias, iclr_up, gate_up, lnx_g, lnx_b, attn_sink, w_out,
              w_router, expert_bias, e_gate, e_up, e_down, s_gate, s_up, s_down, final_g):
    b, t, d = x.shape
    rows = t // GRID_W
    row = jnp.repeat(jnp.arange(rows, dtype=jnp.float32), GRID_W)
    col = jnp.tile(jnp.arange(GRID_W, dtype=jnp.float32), rows)
    for layer in range(DEPTH):
        update_ctx = layer < DEPTH - 1
        mod_x = (jax.nn.silu(c) @ w_mod[layer] + b_mod[layer])[:, None, :]
        mod_c = jax.nn.silu(c_ctx) @ w_mod[layer] + b_mod[layer]
        sh1, sc1, gt1, sh2, sc2, gt2 = jnp.split(mod_x, 6, axis=-1)
        csh1, csc1, cgt1, csh2, csc2, cgt2 = jnp.split(mod_c, 6, axis=-1)
        hx = modulate(rmsnorm(x, norm1_g[layer]), sh1, sc1)
        hc = modulate(rmsnorm(ctx, norm1_g[layer]), csh1, csc1)
        mix_x, mix_c = token_mixers(hx @ w_in[layer], hc @ w_in[layer], row, col, update_ctx,
                                    shift_mu[layer], k_k[layer], k_a[layer], r_k[layer],
                                    decay_bias[layer], decay_up[layer], iclr_bias[layer], iclr_up[layer],
                                    gate_up[layer], lnx_g[layer], lnx_b[layer], attn_sink[layer])
        x = x + gt1 * (mix_x @ w_out[layer])
        h2 = modulate(rmsnorm(x, norm2_g[layer]), sh2, sc2)
        moe_w = (w_router[layer], expert_bias[layer], e_gate[layer], e_up[layer], e_down[layer],
                 s_gate[layer], s_up[layer], s_down[layer])
        if update_ctx:
            ctx = ctx + cgt1 * (mix_c @ w_out[layer])
            hc2 = modulate(rmsnorm(ctx, norm2_g[layer]), csh2, csc2)
            f = moe_ffn(jnp.concatenate([h2.reshape(-1, d), hc2.reshape(-1, d)], axis=0), *moe_w)
            x = x + gt2 * f[:b * t].reshape(x.shape)
            ctx = ctx + cgt2 * f[b * t:].reshape(ctx.shape)
        else:
            x = x + gt2 * moe_ffn(h2.reshape(-1, d), *moe_w).reshape(x.shape)
    return rmsnorm(x, final_g)
```

```python
from contextlib import ExitStack
import concourse.bass as bass
import concourse.mybir as mybir

F32 = mybir.dt.float32
BF16 = mybir.dt.bfloat16
AF = mybir.ActivationFunctionType
ALU = mybir.AluOpType
AX = mybir.AxisListType

ENGS = ("pe", "act", "dve", "pool", "sp")


class StopBuild(Exception):
    pass


class Buf:
    __slots__ = ("name", "w", "r", "dsem", "dcnt", "excl")

    def __init__(self, name, excl=False):
        self.name = name
        self.excl = excl
        self.w = None
        self.r = {}
        self.dsem = None
        self.dcnt = 0


class Prog:
    def __init__(self, nc):
        self.nc = nc
        self.es = ExitStack()
        self.eng = {"pe": nc.tensor, "act": nc.scalar, "dve": nc.vector,
                    "pool": nc.gpsimd, "sp": nc.sync}
        self.nsem = 0
        self.esem = {}
        self.cnt = {}
        for e in ENGS:
            self.esem[e] = self.new_sem("p_" + e)
            self.cnt[e] = 0
        self.waited = {}
        self.nops = 0

    def new_sem(self, name):
        self.nsem += 1
        return self.es.enter_context(self.nc.semaphore(f"{name}_{self.nsem}"))

    def sb(self, name, shape, dt):
        return self.es.enter_context(self.nc.sbuf_tensor("s_" + name, list(shape), dt))

    def ps(self, name, shape, dt):
        return self.es.enter_context(self.nc.psum_tensor("ps_" + name, list(shape), dt))

    def _waits(self, e, reads, writes):
        best = {}

        def add(tok):
            s, v = tok
            k = id(s)
            if k not in best or best[k][1] < v:
                best[k] = (s, v)
        for b in reads:
            if b.w is not None:
                add(b.w)
        for b in writes:
            if b.w is not None:
                add(b.w)
            for k, tok in b.r.items():
                add(tok)
        en = self.eng[e]
        for k, (s, v) in best.items():
            if e == "pe" and s is self.esem["pe"]:
                continue
            wk = (e, k)
            if self.waited.get(wk, 0) >= v:
                continue
            self.waited[wk] = v
            en.wait_ge(s, v)

    def _mark(self, tok, reads, writes):
        k = id(tok[0])
        for b in reads:
            if b.excl:
                b.w = tok
                b.r = {}
            else:
                b.r[k] = tok
        for b in writes:
            b.w = tok
            b.r = {}

    def op(self, e, fn, reads=(), writes=()):
        self._waits(e, reads, writes)
        if self.cnt[e] >= 30000:
            self.esem[e] = self.new_sem("p_" + e)
            self.cnt[e] = 0
        self.cnt[e] += 1
        tok = (self.esem[e], self.cnt[e])
        fn(self.eng[e]).then_inc(tok[0], 1)
        self._mark(tok, reads, writes)
        self.nops += 1
        if getattr(self, "stop_at", None) is not None and self.nops >= self.stop_at:
            self.stop_at = None
            raise StopBuild()

    def dma(self, e, out, in_, owner, reads=(), writes=()):
        self._waits(e, reads, writes)
        if owner.dsem is None:
            owner.dsem = self.new_sem("d_" + owner.name)
        owner.dcnt += 16
        tok = (owner.dsem, owner.dcnt)
        self.eng[e].dma_start(out=out, in_=in_).then_inc(tok[0], 16)
        self._mark(tok, reads, writes)
        self.nops += 1

    def wait_all(self, e, bufs):
        self._waits(e, bufs, ())

    def close(self):
        self.es.close()

import numpy as np
from concourse.bass_utils import run_bass_kernel_spmd
import ml_dtypes

NORM_EPS = 1e-6
D = 2048
NT_B = 1024
NEXP = 65


def build_phaseB():
    nc = bass.Bass("TRN2", target_bir_lowering=False)
    P = Prog(nc)

    def din(name, shape, dt=F32):
        return nc.dram_tensor(name, list(shape), dt, kind="ExternalInput").ap()
    mixT = din("mixT", [D, NT_B], BF16)
    xtok = din("xtok", [NT_B, D])
    wout = din("wout", [D, D])
    wmodB = din("wmodB", [D, 4 * D])
    ccol = din("ccol", [128, 16])
    bmod_cols = din("bmod_cols", [128, 32])
    bmod_rows = din("bmod_rows", [128, 2 * D])
    g2col = din("g2col", [128, 16])
    wr = din("wr", [D, 64])
    ebias = din("ebias", [128, 64])
    eg = din("eg", [NEXP, D, 512])
    eu = din("eu", [NEXP, D, 512])
    ed = din("ed", [NEXP, 512, D])
    fg = din("fg", [128, D])
    ident_d = din("ident", [128, 128])
    out = nc.dram_tensor("out", [NT_B, D], F32, kind="ExternalOutput").ap()
    xnew_d = nc.dram_tensor("xnew_d", [NT_B, D], F32, kind="Internal").ap()

    ident = P.sb("ident", [128, 128], F32); b_ident = Buf("ident")
    sc = P.sb("sc", [128, 16], F32); b_sc = Buf("sc")
    craw = P.sb("craw", [128, 16], F32); b_craw = Buf("craw")
    b_gt = Buf("gt")
    modc = P.sb("modc", [128, 32], F32); b_modc = Buf("modc")
    bcol = P.sb("bcol", [128, 32], F32); b_bcol = Buf("bcol")
    g2 = P.sb("g2", [128, 16], F32); b_g2 = Buf("g2")
    A2 = P.sb("A2", [128, 16], F32); b_A2 = Buf("A2")
    eb_sb = P.sb("eb_sb", [128, 64], F32); b_eb = Buf("eb")
    st = P.sb("st", [128, 8], F32); b_st = Buf("st")
    gates = P.sb("gates", [128, 8, NEXP], F32); b_gates = Buf("gates")
    h2T = P.sb("h2T", [128, 16, NT_B], BF16); b_h2T = [Buf(f"h2T{t}") for t in range(8)]
    acc = P.sb("acc", [128, 8, D], F32); b_acc = [Buf(f"acc{t}") for t in range(8)]
    actT = P.sb("actT", [128, 4, NT_B], BF16); b_act = [Buf("actA"), Buf("actB")]
    sg = [P.sb(f"sg{i}", [128, 512], F32) for i in range(2)]; b_sg = [Buf("sg0"), Buf("sg1")]
    ARENA = 24576
    arena = P.sb("arena", [128, ARENA], F32)
    pb = [P.ps(f"pb{i}", [128, 512], F32) for i in range(8)]
    b_pb = [Buf(f"pb{i}") for i in range(8)]
    gt2_d = nc.dram_tensor("gt2_d", [128, D], F32, kind="Internal").ap()

    def af(off, n):
        return arena[:, off:off + n]
    gt1 = af(22528, 2048)

    def abf(off, n):
        return arena[:, off:off + n].bitcast(BF16)

    P.dma("sp", ident[:], ident_d[:, :], b_ident, writes=[b_ident])
    P.dma("sp", craw[:], ccol[:, :], b_craw, writes=[b_craw])
    P.dma("sp", bcol[:], bmod_cols[:, :], b_bcol, writes=[b_bcol])
    P.dma("sp", g2[:], g2col[:, :], b_g2, writes=[b_g2])
    P.dma("sp", eb_sb[:], ebias[:, :], b_eb, writes=[b_eb])
    P.op("act", lambda e: e.activation(out=sc[:], in_=craw[:], func=AF.Silu), [b_craw], [b_sc])

    sc_rep = af(0, 2048).rearrange("p (k m) -> p k m", m=128); b_screp = Buf("screp")
    wst = [af(2048 + i * 8192, 8192).rearrange("p (k n) -> p k n", n=512) for i in range(2)]
    b_wst = [Buf("wst0"), Buf("wst1")]
    brow = af(18432, 4096); b_brow = Buf("brow")
    P.dma("sp", brow, bmod_rows[:, :], b_brow, writes=[b_brow])
    P.op("dve", lambda e: e.tensor_copy(out=sc_rep, in_=sc[:].unsqueeze(2).to_broadcast([128, 16, 128])),
         [b_sc], [b_screp])
    wmv = wmodB.rearrange("(kc p) n -> p kc n", p=128)
    b_gt2d = Buf("gt2d")
    for pc in range(16):
        s = pc % 2
        P.dma("sp" if pc % 2 == 0 else "act", wst[s], wmv[:, :, pc * 512:(pc + 1) * 512], b_wst[s], writes=[b_wst[s]])
        seg = pc // 4
        if seg in (0, 3):
            pbk = pc % 2
            for kc in range(16):
                P.op("pe", lambda e, kc=kc, s=s, pbk=pbk: e.matmul(pb[pbk][:], lhsT=sc_rep[:, kc, :], rhs=wst[s][:, kc, :],
                                                                      start=(kc == 0), stop=(kc == 15)),
                     [b_screp, b_wst[s]], [b_pb[pbk]])
            off = (pc % 4) * 512
            dst = gt1 if seg == 0 else acc[:, 0, :]
            dbuf = b_gt if seg == 0 else b_acc[0]
            boff = off + (0 if seg == 0 else D)
            P.op("dve", lambda e, off=off, boff=boff, pbk=pbk, dst=dst: e.tensor_tensor(out=dst[:, off:off + 512], in0=pb[pbk][:],
                                                                    in1=brow[:, boff:boff + 512], op=ALU.add),
                 [b_pb[pbk], b_brow], [dbuf])
        else:
            for cc in range(4):
                j = (seg - 1) * 16 + (pc % 4) * 4 + cc
                for kc in range(16):
                    P.op("pe", lambda e, kc=kc, s=s, cc=cc, j=j: e.matmul(pb[2][:, j:j + 1], lhsT=wst[s][:, kc, cc * 128:(cc + 1) * 128],
                                                                          rhs=sc[:, kc:kc + 1], start=(kc == 0), stop=(kc == 15)),
                         [b_sc, b_wst[s]], [b_pb[2]])
    P.dma("sp", gt2_d[:, :], acc[:, 0, :], b_acc[0], reads=[b_acc[0]], writes=[b_gt2d])
    P.op("dve", lambda e: e.tensor_tensor(out=modc[:], in0=pb[2][:, 0:32], in1=bcol[:], op=ALU.add),
         [b_pb[2], b_bcol], [b_modc])
    P.op("dve", lambda e: e.scalar_tensor_tensor(out=A2[:], in0=modc[:, 16:32], scalar=1.0, in1=g2[:], op0=ALU.add, op1=ALU.mult),
         [b_modc, b_g2], [b_A2])
    prev_ar = [b_screp, b_wst[0], b_wst[1], b_brow]

    mixT_sb = abf(0, 8192).rearrange("p (k t) -> p k t", t=NT_B); b_mix = Buf("mixT")
    woh = abf(8192, 8192).rearrange("p (k n) -> p k n", n=1024); b_woh = Buf("woh")
    wr_sb = af(16384, 1024).rearrange("p (k e) -> p k e", e=64); b_wr = Buf("wr")
    h2f = af(17408, 2048).rearrange("p (k t) -> p k t", t=128); b_h2f = Buf("h2f")
    tmp = af(19456, 512); b_tmp = Buf("tmp")
    rt = af(19968, 512).rearrange("p (a b) -> p a b", b=64); b_rt = Buf("rt")
    rs = af(20480, 64).rearrange("p (a b) -> p a b", b=8); b_rs = Buf("rs")
    P.dma("sp", mixT_sb, mixT.rearrange("(kc p) t -> p kc t", p=128), b_mix, writes=[b_mix] + prev_ar)
    P.dma("sp", wr_sb, wr.rearrange("(kc p) e -> p kc e", p=128), b_wr, writes=[b_wr] + prev_ar)
    for tt in range(8):
        P.dma("act", acc[:, tt, :], xtok[tt * 128:(tt + 1) * 128, :], b_acc[tt], reads=[b_gt2d] if tt == 0 else [], writes=[b_acc[tt]])
    P.op("pool", lambda e: e.memset(gates[:], 1.0), [], [b_gates])
    wov = wout.rearrange("(kc p) n -> p kc n", p=128)
    for half in range(2):
        for kq in range(4):
            P.dma("pool", woh[:, kq * 4:(kq + 1) * 4, :], wov[:, kq * 4:(kq + 1) * 4, half * 1024:(half + 1) * 1024], b_woh,
                  writes=[b_woh] + prev_ar)
        for tt in range(8):
            for d2 in range(2):
                pk = d2
                col = half * 1024 + d2 * 512
                for kc in range(16):
                    P.op("pe", lambda e, kc=kc, pk=pk, d2=d2, tt=tt: e.matmul(pb[pk][:], lhsT=mixT_sb[:, kc, tt * 128:(tt + 1) * 128],
                                                                              rhs=woh[:, kc, d2 * 512:(d2 + 1) * 512],
                                                                              start=(kc == 0), stop=(kc == 15)),
                         [b_mix, b_woh], [b_pb[pk]])
                P.op("dve", lambda e, pk=pk, col=col: e.tensor_tensor(out=tmp, in0=pb[pk][:], in1=gt1[:, col:col + 512], op=ALU.mult),
                     [b_pb[pk], b_gt], [b_tmp] + prev_ar)
                P.op("dve", lambda e, col=col, tt=tt: e.tensor_tensor(out=acc[:, tt, col:col + 512], in0=acc[:, tt, col:col + 512],
                                                                      in1=tmp, op=ALU.add),
                     [b_tmp, b_acc[tt]], [b_acc[tt]])

    def rstd_from_ssq(ssq_ap, out_ap, rd, wr_):
        P.op("dve", lambda e: e.tensor_scalar(out=out_ap, in0=ssq_ap, scalar1=1.0 / D, scalar2=NORM_EPS, op0=ALU.mult, op1=ALU.add), rd, wr_)
        P.op("act", lambda e: e.activation(out=out_ap, in_=out_ap, func=AF.Sqrt), wr_, wr_)
        P.op("dve", lambda e: e.reciprocal(out=out_ap, in_=out_ap), wr_, wr_)

    b_xnewd = [Buf(f"xnewd{t}") for t in range(8)]
    h2f_flat = af(17408, 2048)
    for tt in range(8):
        P.dma("sp", xnew_d[tt * 128:(tt + 1) * 128, :], acc[:, tt, :], b_acc[tt], reads=[b_acc[tt]], writes=[b_xnewd[tt]])
        P.op("act", lambda e, tt=tt: e.activation(out=h2f_flat, in_=acc[:, tt, :], func=AF.Square, accum_out=st[:, 0:1]),
             [b_acc[tt]], [b_h2f, b_st] + prev_ar)
        rstd_from_ssq(st[:, 0:1], st[:, 1:2], [b_st], [b_st])
        P.op("dve", lambda e, tt=tt: e.tensor_scalar(out=acc[:, tt, :], in0=acc[:, tt, :], scalar1=st[:, 1:2], scalar2=None, op0=ALU.mult),
             [b_acc[tt], b_st], [b_acc[tt]])
        for dc in range(16):
            pk = 2 + dc % 2
            P.op("pe", lambda e, dc=dc, pk=pk, tt=tt: e.transpose(out=pb[pk][:, 0:128], in_=acc[:, tt, dc * 128:(dc + 1) * 128], identity=ident[:]),
                 [b_acc[tt], b_ident], [b_pb[pk]])
            P.op("act", lambda e, dc=dc, pk=pk: e.activation(out=h2f[:, dc, :], in_=pb[pk][:, 0:128], func=AF.Identity,
                                                             bias=modc[:, dc:dc + 1], scale=A2[:, dc:dc + 1]),
                 [b_pb[pk], b_modc, b_A2], [b_h2f])
        P.op("pool", lambda e, tt=tt: e.memset(acc[:, tt, :], 0.0), [], [b_acc[tt]])
        P.op("dve", lambda e, tt=tt: e.tensor_copy(out=h2T[:, :, tt * 128:(tt + 1) * 128], in_=h2f), [b_h2f], [b_h2T[tt]])
        for dc in range(16):
            P.op("pe", lambda e, dc=dc: e.matmul(pb[4][:, 0:64], lhsT=h2f[:, dc, :], rhs=wr_sb[:, dc, :], start=(dc == 0), stop=(dc == 15)),
                 [b_h2f, b_wr], [b_pb[4]])
        scr = rt[:, 0, :]; bia = rt[:, 1, :]; eq = rt[:, 2, :]; m2 = rt[:, 3, :]; msk = rt[:, 4, :]; sel = rt[:, 5, :]; gs_ = rt[:, 6, :]
        m1 = rs[:, 0, :]; mm2 = rs[:, 1, :]; gsc = rs[:, 2, :]; top = rs[:, 3, :]; gmk = rs[:, 4, :]; pen = rs[:, 5, :]; top2 = rs[:, 6, :]
        R = [b_rt, b_rs]
        P.op("act", lambda e: e.activation(out=scr, in_=pb[4][:, 0:64], func=AF.Sigmoid), [b_pb[4]], R + prev_ar)
        P.op("dve", lambda e: e.tensor_tensor(out=bia, in0=scr, in1=eb_sb[:], op=ALU.add), R + [b_eb], R)
        v3 = lambda a: a.rearrange("p (g k) -> p g k", k=8)
        bc3 = lambda a: a.unsqueeze(2).to_broadcast([128, 8, 8])
        P.op("dve", lambda e: e.tensor_reduce(out=m1, in_=v3(bia), axis=AX.X, op=ALU.max), R, R)
        P.op("dve", lambda e: e.tensor_tensor(out=v3(eq), in0=v3(bia), in1=bc3(m1), op=ALU.is_equal), R, R)
        P.op("dve", lambda e: e.scalar_tensor_tensor(out=m2, in0=eq, scalar=-1e30, in1=bia, op0=ALU.mult, op1=ALU.add), R, R)
        P.op("dve", lambda e: e.tensor_reduce(out=mm2, in_=v3(m2), axis=AX.X, op=ALU.max), R, R)
        P.op("dve", lambda e: e.tensor_tensor(out=gsc, in0=m1, in1=mm2, op=ALU.add), R, R)
        P.op("dve", lambda e: e.max(out=top, in_=gsc), R, R)
        P.op("dve", lambda e: e.tensor_scalar(out=gmk, in0=gsc, scalar1=top[:, 3:4], scalar2=None, op0=ALU.is_ge), R, R)
        P.op("dve", lambda e: e.tensor_scalar(out=pen, in0=gmk, scalar1=-1.0, scalar2=1e30, op0=ALU.add, op1=ALU.mult), R, R)
        P.op("dve", lambda e: e.tensor_tensor(out=v3(msk), in0=v3(bia), in1=bc3(pen), op=ALU.add), R, R)
        P.op("dve", lambda e: e.max(out=top2, in_=msk), R, R)
        P.op("dve", lambda e: e.tensor_scalar(out=sel, in0=msk, scalar1=top2[:, 7:8], scalar2=None, op0=ALU.is_ge), R, R)
        P.op("dve", lambda e: e.tensor_tensor(out=gs_, in0=sel, in1=scr, op=ALU.mult), R, R)
        P.op("dve", lambda e: e.tensor_reduce(out=st[:, 2:3], in_=gs_, axis=AX.X, op=ALU.add), R + [b_st], [b_st])
        P.op("dve", lambda e: e.reciprocal(out=st[:, 3:4], in_=st[:, 2:3]), [b_st], [b_st])
        P.op("dve", lambda e, tt=tt: e.tensor_scalar(out=gates[:, tt, 0:64], in0=gs_, scalar1=st[:, 3:4], scalar2=2.5,
                                                     op0=ALU.mult, op1=ALU.mult), R + [b_st], [b_gates])
    prev_ar = [b_mix, b_woh, b_wr, b_h2f, b_tmp, b_rt, b_rs, b_gt]

    bfview = abf
    wg = [bfview(i * 4096, 4096).rearrange("p (k f) -> p k f", f=512) for i in range(2)]
    wu = [bfview(8192 + i * 4096, 4096).rearrange("p (k f) -> p k f", f=512) for i in range(2)]
    wd = [bfview(16384 + i * 4096, 4096).rearrange("p (k n) -> p k n", n=D) for i in range(2)]
    b_wg = [Buf("wg0"), Buf("wg1")]; b_wu = [Buf("wu0"), Buf("wu1")]; b_wd = [Buf("wd0"), Buf("wd1")]
    alias2 = prev_ar
    egv = eg.rearrange("e (kc p) f -> e p kc f", p=128)
    euv = eu.rearrange("e (kc p) f -> e p kc f", p=128)
    edv = ed.rearrange("e (fc p) n -> e p fc n", p=128)
    for ex in range(NEXP):
        s = ex % 2
        al = alias2 if ex < 2 else []
        for h in range(2):
            P.dma("pool", wg[s][:, h * 8:(h + 1) * 8, :], egv[ex, :, h * 8:(h + 1) * 8, :], b_wg[s], writes=[b_wg[s]] + al)
            P.dma("pool", wu[s][:, h * 8:(h + 1) * 8, :], euv[ex, :, h * 8:(h + 1) * 8, :], b_wu[s], writes=[b_wu[s]] + al)
        for h in range(2):
            P.dma("pool", wd[s][:, h * 2:(h + 1) * 2, :], edv[ex, :, h * 2:(h + 1) * 2, :], b_wd[s], writes=[b_wd[s]] + al)
        for tb in range(2):
            for fc in range(4):
                i = (tb * 4 + fc) % 2
                pg, pu = 0 + i, 2 + i
                for kc in range(16):
                    P.op("pe", lambda e, kc=kc, s=s, fc=fc, tb=tb, pg=pg: e.matmul(pb[pg][:], lhsT=wg[s][:, kc, fc * 128:(fc + 1) * 128],
                                                                                   rhs=h2T[:, kc, tb * 512:(tb + 1) * 512],
                                                                                   start=(kc == 0), stop=(kc == 15)),
                         [b_wg[s]] + b_h2T[tb * 4:(tb + 1) * 4], [b_pb[pg]])
                for kc in range(16):
                    P.op("pe", lambda e, kc=kc, s=s, fc=fc, tb=tb, pu=pu: e.matmul(pb[pu][:], lhsT=wu[s][:, kc, fc * 128:(fc + 1) * 128],
                                                                                   rhs=h2T[:, kc, tb * 512:(tb + 1) * 512],
                                                                                   start=(kc == 0), stop=(kc == 15)),
                         [b_wu[s]] + b_h2T[tb * 4:(tb + 1) * 4], [b_pb[pu]])
                P.op("act", lambda e, i=i, pg=pg: e.activation(out=sg[i][:], in_=pb[pg][:], func=AF.Silu), [b_pb[pg]], [b_sg[i]])
                P.op("dve", lambda e, i=i, pu=pu, fc=fc, tb=tb: e.tensor_tensor(out=actT[:, fc, tb * 512:(tb + 1) * 512], in0=sg[i][:],
                                                                                in1=pb[pu][:], op=ALU.mult),
                     [b_sg[i], b_pb[pu]], [b_act[tb]])
        for tt in range(8):
            for db in range(4):
                py = 4 + (tt * 4 + db) % 4
                for fc in range(4):
                    P.op("pe", lambda e, fc=fc, tt=tt, db=db, s=s, py=py: e.matmul(pb[py][:], lhsT=actT[:, fc, tt * 128:(tt + 1) * 128],
                                                                                   rhs=wd[s][:, fc, db * 512:(db + 1) * 512],
                                                                                   start=(fc == 0), stop=(fc == 3)),
                         [b_act[tt // 4], b_wd[s]], [b_pb[py]])
                P.op("dve", lambda e, tt=tt, db=db, py=py, ex=ex: e.scalar_tensor_tensor(
                    out=acc[:, tt, db * 512:(db + 1) * 512], in0=pb[py][:], scalar=gates[:, tt, ex:ex + 1],
                    in1=acc[:, tt, db * 512:(db + 1) * 512], op0=ALU.mult, op1=ALU.add),
                    [b_pb[py], b_gates, b_acc[tt]], [b_acc[tt]])

    prev_ar = b_wg + b_wu + b_wd
    xt5 = af(0, 2048); fg_sb = af(2048, 2048); gt2 = af(4096, 2048); xo = af(6144, 2048); xf = af(8192, 2048); jk = af(10240, 2048)
    b_xt5 = Buf("xt5"); b_fg = Buf("fg"); b_gt2 = Buf("gt2"); b_xo = Buf("xo"); b_xf = Buf("xf"); b_jk = Buf("jk")
    P.dma("sp", fg_sb, fg[:, :], b_fg, writes=[b_fg] + prev_ar)
    P.dma("sp", gt2, gt2_d[:, :], b_gt2, reads=[b_gt2d], writes=[b_gt2] + prev_ar)
    b_out = Buf("outd")
    for tt in range(8):
        P.dma("sp", xt5, xnew_d[tt * 128:(tt + 1) * 128, :], b_xt5, reads=[b_xnewd[tt]], writes=[b_xt5] + prev_ar)
        P.op("dve", lambda e, tt=tt: e.tensor_tensor(out=xf, in0=acc[:, tt, :], in1=gt2, op=ALU.mult),
             [b_acc[tt], b_gt2], [b_xf] + prev_ar)
        P.op("pool", lambda e: e.tensor_tensor(out=xf, in0=xf, in1=xt5, op=ALU.add), [b_xf, b_xt5], [b_xf])
        P.op("act", lambda e: e.activation(out=jk, in_=xf, func=AF.Square, accum_out=st[:, 4:5]), [b_xf], [b_jk, b_st] + prev_ar)
        rstd_from_ssq(st[:, 4:5], st[:, 5:6], [b_st], [b_st])
        P.op("dve", lambda e: e.scalar_tensor_tensor(out=xo, in0=xf, scalar=st[:, 5:6], in1=fg_sb, op0=ALU.mult, op1=ALU.mult),
             [b_xf, b_st, b_fg], [b_xo] + prev_ar)
        P.dma("sp", out[tt * 128:(tt + 1) * 128, :], xo, b_xo, reads=[b_xo], writes=[b_out])
    P.wait_all("sp", [b_out])
    P.close()
    return nc


T_ALL = 4352
NCH = 14
NCOL = NCH * 128
LNX_EPS = 64e-5


def build_phaseA():
    nc = bass.Bass("TRN2", target_bir_lowering=False)
    P = Prog(nc)

    def din(name, shape, dt=F32):
        return nc.dram_tensor(name, list(shape), dt, kind="ExternalInput").ap()
    import os as _os
    FAST = _os.environ.get("PA_FAST") == "1"
    xT = din("xT", [D, T_ALL] if not FAST else [D, 4])
    cc = din("cc", [128, 32])
    wmodA = din("wmodA", [D, 2 * D] if not FAST else [D, 4])
    bmodA = din("bmodA", [128, 32])
    g1col = din("g1col", [128, 16])
    win = din("win", [D, NCOL] if not FAST else [D, 4])
    mu_d = din("mu", [128, 10])
    pcol_d = din("pcol", [128, 10])
    dbias_d = din("dbias", [128, 4])
    ibias_d = din("ibias", [128, 4])
    dup_d = din("dup", [96, 512])
    iup_d = din("iup", [96, 512])
    gup_d = din("gup", [128, 512])
    sink_d = din("sink", [128, 4])
    cst_d = din("cst", [128, 8 * 128])
    cos_d = din("cos", [128, 4096])
    sin_d = din("sin", [128, 4096])
    mixo = nc.dram_tensor("mixo", [512, 4096], BF16, kind="ExternalOutput").ap()
    pxT = nc.dram_tensor("pxT", [NCOL, T_ALL], F32, kind="Internal").ap()

    cst = P.sb("cst", [128, 8 * 128], F32); b_cst = Buf("cst")
    P.dma("sp", cst[:], cst_d[:, :], b_cst, writes=[b_cst])
    ident = cst[:, 0:128]; SL = cst[:, 128:256]; SU = cst[:, 256:384]; LI = cst[:, 384:512]; UI = cst[:, 512:640]
    bones = cst[:, 640:768]; ropeP = cst[:, 768:896]; ones_f = cst[:, 896:1024]
    small = P.sb("small", [128, 256], F32); b_small = Buf("small")
    ARENA = 47000
    arena = P.sb("arena", [128, ARENA], F32)
    pbk = [P.ps(f"pb{i}", [128, 512], F32) for i in range(8)]
    b_bank = [Buf(f"bank{i}", excl=True) for i in range(8)]
    b_pb = b_bank

    def af(off, n):
        return arena[:, off:off + n]

    def abf(off, n):
        return arena[:, off:off + n].bitcast(BF16)

    ccs = small[:, 0:32]; modA = small[:, 32:96]; g1 = small[:, 96:112]; A1 = small[:, 112:144]; bm = small[:, 144:176]
    mu = small[:, 176:186]; hmu = small[:, 186:196]; omm = small[:, 196:206]; stc = small[:, 206:216]
    pcol = P.sb("pcol", [128, 32], F32); b_pcol = Buf("pcol")
    P.dma("sp", ccs, cc[:, :], b_small, writes=[b_small])
    P.dma("sp", g1, g1col[:, :], b_small, writes=[b_small])
    P.dma("sp", bm, bmodA[:, :], b_small, writes=[b_small])
    P.dma("sp", mu, mu_d[:, :], b_small, writes=[b_small])
    P.dma("sp", pcol[:, 0:10], pcol_d[:, :], b_pcol, writes=[b_pcol])
    P.dma("sp", pcol[:, 10:14], dbias_d[:, :], b_pcol, writes=[b_pcol])
    P.dma("sp", pcol[:, 14:18], ibias_d[:, :], b_pcol, writes=[b_pcol])
    P.dma("sp", pcol[:, 18:22], sink_d[:, :], b_pcol, writes=[b_pcol])
    S_ = [b_small]
    P.op("act", lambda e: e.activation(out=ccs, in_=ccs, func=AF.Silu), S_, S_)
    P.op("dve", lambda e: e.tensor_scalar(out=hmu, in0=mu, scalar1=0.5, scalar2=None, op0=ALU.mult), S_, S_)
    P.op("dve", lambda e: e.tensor_scalar(out=omm, in0=mu, scalar1=-1.0, scalar2=1.0, op0=ALU.mult, op1=ALU.add), S_, S_)
    for hp in range(2):
        P.op("dve", lambda e, hp=hp: e.tensor_scalar(out=pcol[:, 22 + hp:23 + hp], in0=pcol[:, hp * 5 + 1:hp * 5 + 2], scalar1=-1.0, scalar2=1.0,
                                                     op0=ALU.mult, op1=ALU.add), [b_pcol], [b_pcol])
    P.op("act", lambda e: e.activation(out=pcol[:, 24:28], in_=pcol[:, 18:22], func=AF.Exp), [b_pcol], [b_pcol])

    wst = [af(i * 8192, 8192).rearrange("p (k n) -> p k n", n=512) for i in range(2)]
    b_wst = [Buf("wst0"), Buf("wst1")]
    wmv = wmodA.rearrange("(kc p) n -> p kc n", p=128) if not FAST else None
    ccv = ccs.rearrange("p (k two) -> p k two", two=2)
    for pc in range(8 if not FAST else 0):
        s = pc % 2
        P.dma("sp" if s == 0 else "act", wst[s], wmv[:, :, pc * 512:(pc + 1) * 512], b_wst[s], writes=[b_wst[s]])
        for c4 in range(4):
            j = pc * 4 + c4
            for kc in range(16):
                P.op("pe", lambda e, kc=kc, s=s, c4=c4, j=j: e.matmul(pbk[0][:, 2 * j:2 * j + 2], lhsT=wst[s][:, kc, c4 * 128:(c4 + 1) * 128],
                                                                      rhs=ccv[:, kc, :], start=(kc == 0), stop=(kc == 15)),
                     [b_small, b_wst[s]], [b_pb[0]])
    P.op("dve", lambda e: e.tensor_tensor(out=modA.rearrange("p (j w) -> p j w", w=2), in0=pbk[0][:, 0:64].rearrange("p (j w) -> p j w", w=2),
                                          in1=bm.unsqueeze(2).to_broadcast([128, 32, 2]), op=ALU.add), [b_pb[0], b_small], S_)
    modv = modA.rearrange("p (j w) -> p j w", w=2)
    A1v = A1.rearrange("p (j w) -> p j w", w=2)
    P.op("dve", lambda e: e.scalar_tensor_tensor(out=A1v, in0=modv[:, 16:32, :], scalar=1.0, in1=g1.unsqueeze(2).to_broadcast([128, 16, 2]),
                                                 op0=ALU.add, op1=ALU.mult), S_, S_)
    prev_ar = list(b_wst)

    win_bf = abf(0, 14336).rearrange("p (k n) -> p k n", n=NCOL); b_win = Buf("win")
    xin = af(14336, 8192).rearrange("p (k t) -> p k t", t=512); b_xin = Buf("xin")
    sq = abf(22528, 4096).rearrange("p (k t) -> p k t", t=512); b_sq = Buf("sq")
    hx = abf(26624, 4096).rearrange("p (k t) -> p k t", t=512); b_hx = Buf("hx")
    rstd = af(30720, 512); b_rstd = Buf("rstd")
    tmpx = af(31232, 512); b_tmpx = Buf("tmpx")
    ev = [af(31744 + i * 512, 512) for i in range(2)]; b_ev = [Buf("ev0"), Buf("ev1")]
    ones_bf = abf(32768, 64); b_ones = Buf("ones")
    wiv = win.rearrange("(kc p) n -> p kc n", p=128) if not FAST else None
    for kq in range(4 if not FAST else 0):
        P.dma("pool", win_bf[:, kq * 4:(kq + 1) * 4, :], wiv[:, kq * 4:(kq + 1) * 4, :], b_win, writes=[b_win] + prev_ar)
    P.op("pool", lambda e: e.memset(ones_bf, 1.0), [], [b_ones])
    xTv = xT.rearrange("(kc p) t -> p kc t", p=128) if not FAST else None
    b_px = [Buf(f"px{c}") for c in range(NCH)]
    blocks = [(0, 256)] + [(256 + i * 512, 512) for i in range(8)]
    if FAST:
        blocks = []
    for bi, (t0, nt) in enumerate(blocks):
        w = 1 if bi == 0 else 0
        P.dma("sp", xin[:, :, 0:nt], xTv[:, :, t0:t0 + nt], b_xin, writes=[b_xin] + prev_ar)
        P.op("act", lambda e, nt=nt: e.activation(out=sq[:, :, 0:nt], in_=xin[:, :, 0:nt], func=AF.Square), [b_xin], [b_sq] + prev_ar)
        for kc in range(16):
            P.op("pe", lambda e, kc=kc, nt=nt: e.matmul(pbk[1][:, 0:nt], lhsT=ones_bf, rhs=sq[:, kc, 0:nt], start=(kc == 0), stop=(kc == 15)),
                 [b_sq, b_ones], [b_pb[1]])
        P.op("dve", lambda e, nt=nt: e.tensor_scalar(out=rstd[:, 0:nt], in0=pbk[1][:, 0:nt], scalar1=1.0 / D, scalar2=NORM_EPS,
                                                     op0=ALU.mult, op1=ALU.add), [b_pb[1]], [b_rstd])
        P.op("act", lambda e, nt=nt: e.activation(out=rstd[:, 0:nt], in_=rstd[:, 0:nt], func=AF.Sqrt), [b_rstd], [b_rstd])
        P.op("dve", lambda e, nt=nt: e.reciprocal(out=rstd[:, 0:nt], in_=rstd[:, 0:nt]), [b_rstd], [b_rstd])
        for kc in range(16):
            P.op("dve", lambda e, kc=kc, nt=nt, w=w: e.scalar_tensor_tensor(out=tmpx[:, 0:nt], in0=xin[:, kc, 0:nt], scalar=A1v[:, kc, w:w + 1],
                                                                          in1=rstd[:, 0:nt], op0=ALU.mult, op1=ALU.mult),
                 [b_xin, b_rstd, b_small], [b_tmpx])
            P.op("act", lambda e, kc=kc, nt=nt, w=w: e.activation(out=hx[:, kc, 0:nt], in_=tmpx[:, 0:nt], func=AF.Identity,
                                                                 bias=modv[:, kc, w:w + 1], scale=1.0), [b_tmpx, b_small], [b_hx])
        for c in range(NCH):
            pk = 2 + c % 2
            for kc in range(16):
                P.op("pe", lambda e, kc=kc, c=c, nt=nt, pk=pk: e.matmul(pbk[pk][:, 0:nt], lhsT=win_bf[:, kc, c * 128:(c + 1) * 128], rhs=hx[:, kc, 0:nt],
                                                                        start=(kc == 0), stop=(kc == 15)), [b_win, b_hx], [b_pb[pk]])
            i = c % 2
            if i == 0:
                P.op("act", lambda e, nt=nt, pk=pk, i=i: e.activation(out=ev[i][:, 0:nt], in_=pbk[pk][:, 0:nt], func=AF.Identity), [b_pb[pk]], [b_ev[i]])
            else:
                P.op("dve", lambda e, nt=nt, pk=pk, i=i: e.tensor_copy(out=ev[i][:, 0:nt], in_=pbk[pk][:, 0:nt]), [b_pb[pk]], [b_ev[i]])
            P.dma("sp", pxT[c * 128:(c + 1) * 128, t0:t0 + nt], ev[i][:, 0:nt], b_ev[i], reads=[b_ev[i]], writes=[b_px[c]])
    prev_ar = [b_win, b_xin, b_sq, b_hx, b_rstd, b_tmpx, b_ev[0], b_ev[1], b_ones]

    import os as _os
    if _os.environ.get("PA_STOP") == "1":
        P.wait_all("sp", b_px)
        P.close()
        return nc
    T = T_ALL
    rS = af(0, T); kS = af(4352, T); vS = af(8704, T); kk = af(13056, T); ksum = af(17408, T)
    ysum = af(21760, 4096)
    Praw = af(25856, 4356); tmpT = af(30212, 4096)
    tw_bf = abf(34308, 2176); xa_bf = abf(36484, 2176)
    sgx = abf(38660, 4096).rearrange("p (k t) -> p k t", t=4096)
    b_rS = Buf("rS"); b_kS = Buf("kS"); b_vS = Buf("vS"); b_kk = Buf("kk"); b_ksum = Buf("ksum"); b_ysum = Buf("ysum")
    b_praw = Buf("praw"); b_tmpT = Buf("tmpT"); b_tw = Buf("tw"); b_xa = Buf("xa"); b_sgx = Buf("sgx")
    tile_offs = [25856 + i * 128 for i in range(66)] + [42756 + i * 128 for i in range(33)]
    names = ['sgw', 'ag', 'lw', 'c', 'cp', 'ec', 'enc', 'ecp', 'At', 'Rt', 'bb', 'Bt', 'kd', 'Kt']
    znames = ['Bz', 'Kz', 'Vz', 'Uz', 'SAz']
    hnames = ['Q0', 'Q1', 'P0', 'P1', 'X0', 'X1', 'Mak', 'Nrb', 'Nrk']
    TL = [dict(), dict()]
    ZOUT = {}
    BL = [dict(), dict()]
    ti = 0
    for s in range(2):
        for n in names:
            TL[s][n] = af(tile_offs[ti], 128); BL[s][n] = Buf(f"{n}{s}"); ti += 1
        for n in znames:
            TL[s][n] = arena[:, tile_offs[ti]:tile_offs[ti] + 256].rearrange("p (h c) -> p h c", c=128) if tile_offs[ti + 1] == tile_offs[ti] + 128 else None
            assert TL[s][n] is not None
            BL[s][n] = Buf(f"{n}{s}")
            ZOUT[id(BL[s][n])] = arena[:, tile_offs[ti]:tile_offs[ti] + 384].rearrange("p (h c) -> p h c", c=192)[:, :, 0:64]
            ti += 2
        for h in range(2):
            for n in hnames:
                TL[s][n + str(h)] = af(tile_offs[ti], 128); BL[s][n + str(h)] = Buf(f"{n}{h}{s}"); ti += 1
    Sw = af(tile_offs[ti], 128); b_Sw = Buf("Sw"); ti += 1
    tmpS = af(tile_offs[ti], 128); b_tmpS = Buf("tmpS"); ti += 1
    gcol = af(tile_offs[ti], 128); b_gcol = Buf("gcol"); ti += 1
    assert ti <= 99
    blk_bufs = [b for s in range(2) for b in BL[s].values()] + [b_Sw, b_tmpS, b_gcol]
    shift_bufs = [b_praw, b_tmpT]
    def _bank_of(i):
        return (i // 16) * 4 + (i % 4)
    slots = [pbk[_bank_of(i)][:, ((i % 16) // 4) * 128:((i % 16) // 4 + 1) * 128] for i in range(32)]
    b_slots = [b_bank[_bank_of(i)] for i in range(32)]
    ring = [0, 0]

    def nslot(s):
        i = s * 16 + ring[s] % 16
        ring[s] += 1
        return slots[i], b_slots[i]

    P.op("pool", lambda e: e.memset(Praw[:, 0:1], 0.0), [], [b_praw] + prev_ar)
    P.op("pool", lambda e: e.memset(Praw[:, 257:259], 0.0), [], [b_praw])
    P.op("pool", lambda e: e.memset(Praw[:, 4355:4356], 0.0), [], [b_praw])

    def shift_chunk(ch, dst, b_dst, extra_w):
        wl = [b_praw] + extra_w
        P.dma("sp", Praw[:, 1:257], pxT[ch * 128:(ch + 1) * 128, 0:256], b_praw, reads=[b_px[ch]], writes=wl)
        P.dma("act", Praw[:, 259:4355], pxT[ch * 128:(ch + 1) * 128, 256:T], b_praw, reads=[b_px[ch]], writes=wl)
        for (lo, n, plo) in ((0, 256, 1), (256, 4096, 259)):
            P.op("dve", lambda e, n=n, plo=plo: e.tensor_tensor(out=tmpT[:, 0:n], in0=Praw[:, plo - 1:plo - 1 + n], in1=Praw[:, plo + 1:plo + 1 + n], op=ALU.add),
                 [b_praw], [b_tmpT] + extra_w)
            P.op("act", lambda e, lo=lo, n=n, plo=plo: e.activation(out=dst[:, lo:lo + n], in_=Praw[:, plo:plo + n], func=AF.Identity, scale=omm[:, ch:ch + 1]),
                 [b_praw, b_small], [b_dst] + extra_w)
            P.op("dve", lambda e, lo=lo, n=n: e.scalar_tensor_tensor(out=dst[:, lo:lo + n], in0=tmpT[:, 0:n], scalar=hmu[:, ch:ch + 1], in1=dst[:, lo:lo + n],
                                                                     op0=ALU.mult, op1=ALU.add), [b_tmpT, b_dst, b_small], [b_dst])

    shift_chunk(6, ksum, b_ksum, prev_ar)
    P.op("act", lambda e: e.activation(out=tw_bf, in_=ksum, func=AF.Tanh), [b_ksum], [b_tw] + prev_ar)
    shift_chunk(7, ksum, b_ksum, [])
    P.op("act", lambda e: e.activation(out=xa_bf, in_=ksum, func=AF.Identity), [b_ksum], [b_xa] + prev_ar)
    for kc in range(2):
        shift_chunk(8 + kc, ksum, b_ksum, [])
        P.op("act", lambda e, kc=kc: e.activation(out=sgx[:, kc, :], in_=ksum[:, 256:T], func=AF.Sigmoid), [b_ksum], [b_sgx] + prev_ar)
    if _os.environ.get("PA_STOP") == "2a":
        P.wait_all("sp", [])
        for _e in ("act", "dve", "pool", "pe"):
            _b = Buf("fin"); _b.w = (P.esem[_e], P.cnt[_e]); P.wait_all("sp", [_b])
        P.close()
        return nc
    dup_bf = P.sb("dup_bf", [96, 512], BF16); iup_bf = P.sb("iup_bf", [96, 512], BF16); gup_bf = P.sb("gup_bf", [128, 512], BF16)
    b_lora = Buf("lora")
    P.dma("pool", dup_bf[:], dup_d[:, :], b_lora, writes=[b_lora])
    P.dma("pool", iup_bf[:], iup_d[:, :], b_lora, writes=[b_lora])
    P.dma("pool", gup_bf[:], gup_d[:, :], b_lora, writes=[b_lora])
    mixst = [P.sb(f"mixst{i}", [128, 512], BF16) for i in range(2)]; b_mixst = [Buf("mixst0"), Buf("mixst1")]
    b_mixo = Buf("mixo")
    unit_no = [0]

    evc = [0]

    def evac_z(ps, b_ps, zt, b_z, extra):
        zo = zt[:, 0, :].rearrange("p (a c) -> p a c", c=64)
        out3 = ZOUT[id(b_z)]
        in3 = ps.rearrange("p (h c) -> p h c", c=64)
        evc[0] += 1
        if evc[0] % 2 == 0:
            P.op("act", lambda e: e.activation(out=out3, in_=in3, func=AF.Identity), [b_ps], [b_z] + extra)
        else:
            P.op("dve", lambda e: e.tensor_copy(out=out3, in_=in3), [b_ps], [b_z] + extra)

    def unit(hp, d, blk, first_dir):
        if unit_no[0] == 0 and _os.environ.get("PA_UOPS"):
            P.stop_at = P.nops + int(_os.environ["PA_UOPS"])
        s = unit_no[0] % 2
        unit_no[0] += 1
        tl, bl = TL[s], BL[s]
        X = shift_bufs
        c0 = blk * 128
        cs = slice(c0, c0 + 128)
        pc = pcol
        ps, bps = nslot(s)
        P.op("pe", lambda e: e.matmul(ps, lhsT=dup_bf[:, d * 256 + hp * 128:d * 256 + (hp + 1) * 128], rhs=tw_bf[0:96, cs], start=True, stop=True),
             [b_lora, b_tw], [bps])
        P.op("act", lambda e: e.activation(out=tl['sgw'], in_=ps, func=AF.Sigmoid, bias=pc[:, 10 + hp * 2 + d:11 + hp * 2 + d], scale=1.0),
             [bps, b_pcol], [bl['sgw']] + X)
        ps2, bps2 = nslot(s)
        P.op("pe", lambda e: e.matmul(ps2, lhsT=iup_bf[:, d * 256 + hp * 128:d * 256 + (hp + 1) * 128], rhs=xa_bf[0:96, cs], start=True, stop=True),
             [b_lora, b_xa], [bps2])
        P.op("act", lambda e: e.activation(out=tl['ag'], in_=ps2, func=AF.Sigmoid, bias=pc[:, 14 + hp * 2 + d:15 + hp * 2 + d], scale=1.0),
             [bps2, b_pcol], [bl['ag']] + X)
        P.op("dve", lambda e: e.tensor_scalar(out=tl['lw'], in0=tl['sgw'], scalar1=-0.6065306597126334, scalar2=None, op0=ALU.mult),
             [bl['sgw']], [bl['lw']] + X)
        P.op("dve", lambda e: e.tensor_tensor_scan(out=tl['c'], data0=ones_f, data1=tl['lw'], initial=0.0, op0=ALU.mult, op1=ALU.add),
             [bl['lw'], b_cst], [bl['c']] + X)
        P.op("act", lambda e: e.activation(out=gcol[:, 2 * s + 1:2 * s + 2], in_=tl['c'][:, 127:128], func=AF.Exp), [bl['c']], [b_gcol] + X)
        if d == 1:
            P.op("act", lambda e: e.activation(out=gcol[:, 2 * s:2 * s + 1], in_=tl['c'][:, 127:128], func=AF.Identity), [bl['c']], [b_gcol])
            P.op("dve", lambda e: e.tensor_scalar(out=tl['c'], in0=tl['c'], scalar1=-1.0, scalar2=gcol[:, 2 * s:2 * s + 1], op0=ALU.mult, op1=ALU.add),
                 [bl['c'], b_gcol], [bl['c']])
            P.op("dve", lambda e: e.tensor_tensor(out=tl['c'], in0=tl['c'], in1=tl['lw'], op=ALU.add), [bl['c'], bl['lw']], [bl['c']])
        P.op("dve", lambda e: e.tensor_tensor(out=tl['cp'], in0=tl['c'], in1=tl['lw'], op=ALU.subtract), [bl['c'], bl['lw']], [bl['cp']] + X)
        P.op("act", lambda e: e.activation(out=tl['ec'], in_=tl['c'], func=AF.Exp), [bl['c']], [bl['ec']] + X)
        P.op("act", lambda e: e.activation(out=tl['enc'], in_=tl['c'], func=AF.Exp, scale=-1.0), [bl['c']], [bl['enc']] + X)
        P.op("act", lambda e: e.activation(out=tl['ecp'], in_=tl['cp'], func=AF.Exp), [bl['cp']], [bl['ecp']] + X)
        P.op("dve", lambda e: e.scalar_tensor_tensor(out=tl['At'], in0=kk[:, cs], scalar=-1.0, in1=tl['ecp'], op0=ALU.mult, op1=ALU.mult),
             [b_kk, bl['ecp']], [bl['At']] + X)
        P.op("dve", lambda e: e.tensor_tensor(out=tl['Rt'], in0=rS[:, cs], in1=tl['ec'], op=ALU.mult), [b_rS, bl['ec']], [bl['Rt']] + X)
        P.op("dve", lambda e: e.tensor_tensor(out=tl['bb'], in0=kk[:, cs], in1=tl['ag'], op=ALU.mult), [b_kk, bl['ag']], [bl['bb']] + X)
        P.op("dve", lambda e: e.tensor_tensor(out=tl['Bt'], in0=tl['bb'], in1=tl['enc'], op=ALU.mult), [bl['bb'], bl['enc']], [bl['Bt']] + X)
        P.op("dve", lambda e: e.tensor_scalar(out=tl['kd'], in0=tl['ag'], scalar1=pc[:, hp * 5 + 1:hp * 5 + 2], scalar2=pc[:, 22 + hp:23 + hp],
                                              op0=ALU.mult, op1=ALU.add), [bl['ag'], b_pcol], [bl['kd']] + X)
        P.op("dve", lambda e: e.tensor_tensor(out=tl['kd'], in0=tl['kd'], in1=kS[:, cs], op=ALU.mult), [bl['kd'], b_kS], [bl['kd']])
        if blk >= 2:
            if first_dir:
                P.op("pool", lambda e: e.tensor_copy(out=ksum[:, cs], in_=tl['kd']), [bl['kd']], [b_ksum])
            else:
                P.op("pool", lambda e: e.tensor_tensor(out=ksum[:, cs], in0=ksum[:, cs], in1=tl['kd'], op=ALU.add), [bl['kd'], b_ksum], [b_ksum])
        P.op("dve", lambda e: e.tensor_tensor(out=tl['Kt'], in0=tl['kd'], in1=tl['enc'], op=ALU.mult), [bl['kd'], bl['enc']], [bl['Kt']] + X)
        for (src, bsrc, zn) in ((tl['Bt'], bl['Bt'], 'Bz'), (tl['Kt'], bl['Kt'], 'Kz'), (vS[:, cs], b_vS, 'Vz')):
            pt, bpt = nslot(s)
            P.op("pe", lambda e, src=src, pt=pt: e.transpose(out=pt, in_=src, identity=ident), [bsrc, b_cst], [bpt])
            evac_z(pt, bpt, tl[zn], bl[zn], X)
        mM, mMT, mN = (SL, SU, UI) if d == 0 else (SU, SL, LI)
        for h in range(2):
            hs = slice(h * 64, (h + 1) * 64)
            H = str(h)
            pm, bpm = nslot(s)
            P.op("pe", lambda e, pm=pm, hs=hs: e.matmul(pm, lhsT=tl['At'][hs, :], rhs=tl['Bt'][hs, :], start=True, stop=True), [bl['At'], bl['Bt']], [bpm])
            P.op("dve", lambda e, pm=pm, H=H: e.tensor_tensor(out=tl['Q0' + H], in0=pm, in1=mM, op=ALU.mult), [bpm, b_cst], [bl['Q0' + H]] + X)
            pm2, bpm2 = nslot(s)
            P.op("pe", lambda e, pm2=pm2, hs=hs: e.matmul(pm2, lhsT=tl['Bt'][hs, :], rhs=tl['At'][hs, :], start=True, stop=True), [bl['At'], bl['Bt']], [bpm2])
            P.op("dve", lambda e, pm2=pm2, H=H: e.tensor_tensor(out=tl['P0' + H], in0=pm2, in1=mMT, op=ALU.mult), [bpm2, b_cst], [bl['P0' + H]] + X)
            P.op("pool", lambda e, H=H: e.tensor_tensor(out=tl['X0' + H], in0=tl['P0' + H], in1=ident, op=ALU.add), [bl['P0' + H], b_cst], [bl['X0' + H]] + X)
            for j in range(6):
                a, b = str(j % 2), str((j + 1) % 2)
                Qj, Pj, Xj = tl['Q' + a + H], tl['P' + a + H], tl['X' + a + H]
                Qn, Pn, Xn = tl['Q' + b + H], tl['P' + b + H], tl['X' + b + H]
                bQj, bPj, bXj = bl['Q' + a + H], bl['P' + a + H], bl['X' + a + H]
                bQn, bPn, bXn = bl['Q' + b + H], bl['P' + b + H], bl['X' + b + H]
                pq, bpq = nslot(s)
                P.op("pe", lambda e, pq=pq, Pj=Pj, Qj=Qj: e.matmul(pq, lhsT=Pj, rhs=Qj, start=True, stop=True), [bPj, bQj], [bpq])
                P.op("act", lambda e, pq=pq, Qn=Qn: e.activation(out=Qn, in_=pq, func=AF.Identity), [bpq], [bQn])
                if j < 5:
                    pp, bpp = nslot(s)
                    P.op("pe", lambda e, pp=pp, Pj=Pj, Qj=Qj: e.matmul(pp, lhsT=Qj, rhs=Pj, start=True, stop=True), [bPj, bQj], [bpp])
                    P.op("act", lambda e, pp=pp, Pn=Pn: e.activation(out=Pn, in_=pp, func=AF.Identity), [bpp], [bPn])
                px_, bpx = nslot(s)
                P.op("pe", lambda e, px_=px_, Qn=Qn, Xj=Xj: e.matmul(px_, lhsT=Qn, rhs=Xj, start=True, stop=True), [bQn, bXj], [bpx])
                P.op("dve", lambda e, px_=px_, Xn=Xn, Xj=Xj: e.tensor_tensor(out=Xn, in0=px_, in1=Xj, op=ALU.add), [bpx, bXj], [bXn])
            for (nm, lh, rh, mk) in (('Mak', 'Kt', 'At', mMT), ('Nrb', 'Bt', 'Rt', mN), ('Nrk', 'Kt', 'Rt', mN)):
                pn, bpn = nslot(s)
                P.op("pe", lambda e, pn=pn, lh=lh, rh=rh, hs=hs: e.matmul(pn, lhsT=tl[lh][hs, :], rhs=tl[rh][hs, :], start=True, stop=True),
                     [bl[lh], bl[rh]], [bpn])
                P.op("dve", lambda e, pn=pn, nm=nm, H=H, mk=mk: e.tensor_tensor(out=tl[nm + H], in0=pn, in1=mk, op=ALU.mult), [bpn, b_cst], [bl[nm + H]] + X)
        pu, bpu = nslot(s)
        P.op("pe", lambda e: e.matmul(pu, lhsT=tl['At'], rhs=Sw, start=True, stop=False), [bl['At'], b_Sw], [bpu])
        P.op("pe", lambda e: e.matmul(pu, lhsT=tl['Mak0'], rhs=tl['Vz'][:, 0, :], start=False, stop=False), [bl['Mak0'], bl['Vz']], [bpu])
        P.op("pe", lambda e: e.matmul(pu, lhsT=tl['Mak1'], rhs=tl['Vz'][:, 1, :], start=False, stop=True), [bl['Mak1'], bl['Vz']], [bpu])
        evac_z(pu, bpu, tl['Uz'], bl['Uz'], X)
        psa, bpsa = nslot(s)
        P.op("pe", lambda e: e.matmul(psa, lhsT=tl['X00'], rhs=tl['Uz'][:, 0, :], start=True, stop=False), [bl['X00'], bl['Uz']], [bpsa])
        P.op("pe", lambda e: e.matmul(psa, lhsT=tl['X01'], rhs=tl['Uz'][:, 1, :], start=False, stop=True), [bl['X01'], bl['Uz']], [bpsa])
        evac_z(psa, bpsa, tl['SAz'], bl['SAz'], X)
        if blk >= 2:
            py, bpy = nslot(s)
            P.op("pe", lambda e: e.matmul(py, lhsT=Sw, rhs=tl['Rt'], start=True, stop=False), [b_Sw, bl['Rt']], [bpy])
            for h in range(2):
                H = str(h)
                P.op("pe", lambda e, h=h, H=H: e.matmul(py, lhsT=tl['SAz'][:, h, :], rhs=tl['Nrb' + H], start=False, stop=False), [bl['SAz'], bl['Nrb' + H]], [bpy])
                P.op("pe", lambda e, h=h, H=H: e.matmul(py, lhsT=tl['Vz'][:, h, :], rhs=tl['Nrk' + H], start=False, stop=(h == 1)), [bl['Vz'], bl['Nrk' + H]], [bpy])
            yc = slice(c0 - 256, c0 - 128)
            if first_dir:
                P.op("dve", lambda e: e.tensor_copy(out=ysum[:, yc], in_=py), [bpy], [b_ysum])
            else:
                P.op("dve", lambda e: e.tensor_tensor(out=ysum[:, yc], in0=py, in1=ysum[:, yc], op=ALU.add), [bpy, b_ysum], [b_ysum])
        pS, bpS = nslot(s)
        P.op("pe", lambda e: e.matmul(pS, lhsT=tl['Bz'][:, 0, :], rhs=tl['SAz'][:, 0, :], start=True, stop=False), [bl['Bz'], bl['SAz']], [bpS])
        P.op("pe", lambda e: e.matmul(pS, lhsT=tl['Bz'][:, 1, :], rhs=tl['SAz'][:, 1, :], start=False, stop=False), [bl['Bz'], bl['SAz']], [bpS])
        P.op("pe", lambda e: e.matmul(pS, lhsT=tl['Kz'][:, 0, :], rhs=tl['Vz'][:, 0, :], start=False, stop=False), [bl['Kz'], bl['Vz']], [bpS])
        P.op("pe", lambda e: e.matmul(pS, lhsT=tl['Kz'][:, 1, :], rhs=tl['Vz'][:, 1, :], start=False, stop=True), [bl['Kz'], bl['Vz']], [bpS])
        P.op("dve", lambda e: e.tensor_tensor(out=tmpS, in0=pS, in1=Sw, op=ALU.add), [bpS, b_Sw], [b_tmpS] + X)
        P.op("dve", lambda e: e.tensor_scalar(out=Sw, in0=tmpS, scalar1=gcol[:, 2 * s + 1:2 * s + 2], scalar2=None, op0=ALU.mult), [b_tmpS, b_gcol], [b_Sw])

    def _rwkv_all():
        for hp in range(2):
            shift_chunk(0 + hp, rS, b_rS, blk_bufs)
            shift_chunk(2 + hp, kS, b_kS, [])
            shift_chunk(4 + hp, vS, b_vS, [])
            P.op("act", lambda e, hp=hp: e.activation(out=kk, in_=kS, func=AF.Identity, scale=pcol[:, hp * 5:hp * 5 + 1]), [b_kS, b_pcol], [b_kk])
            for (lo, n) in [(i * 512, 512) for i in range(8)] + [(4096, 256)]:
                P.op("act", lambda e, lo=lo, n=n: e.activation(out=tmpT[:, 0:n], in_=kk[:, lo:lo + n], func=AF.Square), [b_kk], [b_tmpT])
                pq_, bpq_ = slots[0], b_slots[0]
                P.op("pe", lambda e, n=n: e.matmul(pbk[0][:, 0:n], lhsT=bones, rhs=tmpT[:, 0:n], start=True, stop=True), [b_tmpT, b_cst], [b_bank[0]])
                P.op("dve", lambda e, n=n: e.tensor_scalar(out=tmpT[:, 0:n], in0=pbk[0][:, 0:n], scalar1=1e-12, scalar2=None, op0=ALU.add), [b_bank[0]], [b_tmpT])
                P.op("act", lambda e, n=n: e.activation(out=tmpT[:, 0:n], in_=tmpT[:, 0:n], func=AF.Sqrt), [b_tmpT], [b_tmpT])
                P.op("dve", lambda e, n=n: e.reciprocal(out=tmpT[:, 0:n], in_=tmpT[:, 0:n]), [b_tmpT], [b_tmpT])
                P.op("dve", lambda e, lo=lo, n=n: e.tensor_tensor(out=kk[:, lo:lo + n], in0=kk[:, lo:lo + n], in1=tmpT[:, 0:n], op=ALU.mult), [b_kk, b_tmpT], [b_kk])
            if _os.environ.get("PA_STOP") == "2b":
                P.wait_all("sp", [])
                for _e in ("act", "dve", "pool", "pe"):
                    _b = Buf("fin"); _b.w = (P.esem[_e], P.cnt[_e]); P.wait_all("sp", [_b])
                pass
                raise StopBuild()
            for s in range(2):
                for zn in znames:
                    P.op("pool", lambda e, s=s, zn=zn: e.memset(TL[s][zn], 0.0), [], [BL[s][zn]] + shift_bufs)
            for d in range(2):
                P.op("pool", lambda e: e.memset(Sw, 0.0), [], [b_Sw] + shift_bufs)
                order = list(range(34)) if d == 0 else [1, 0] + list(range(33, 1, -1))
                for blk in order:
                    unit(hp, d, blk, d == 0)
                    if _os.environ.get("PA_UNITS") and unit_no[0] >= int(_os.environ["PA_UNITS"]):
                        P.wait_all("sp", [])
                        for _e in ("act", "dve", "pool", "pe"):
                            _b = Buf("fin"); _b.w = (P.esem[_e], P.cnt[_e]); P.wait_all("sp", [_b])
                        pass
                        raise StopBuild()
            for pi in range(8):
                xc = slice(pi * 512, (pi + 1) * 512)
                tc_ = slice(256 + pi * 512, 256 + (pi + 1) * 512)
                o1, o2, o3 = tmpT[:, 0:512], tmpT[:, 512:1024], tmpT[:, 1024:1536]
                W_ = [b_tmpT] + blk_bufs
                P.op("pe", lambda e, xc=xc: e.matmul(pbk[0][:], lhsT=bones, rhs=ysum[:, xc], start=True, stop=True), [b_ysum, b_cst], [b_bank[0]])
                P.op("dve", lambda e, xc=xc: e.scalar_tensor_tensor(out=o1, in0=pbk[0][:], scalar=-1.0 / 64, in1=ysum[:, xc], op0=ALU.mult, op1=ALU.add),
                     [b_bank[0], b_ysum], W_)
                P.op("act", lambda e: e.activation(out=o2, in_=o1, func=AF.Square), [b_tmpT], [b_tmpT])
                P.op("pe", lambda e: e.matmul(pbk[1][:], lhsT=bones, rhs=o2, start=True, stop=True), [b_tmpT, b_cst], [b_bank[1]])
                P.op("dve", lambda e: e.tensor_scalar(out=o2, in0=pbk[1][:], scalar1=1.0 / 64, scalar2=LNX_EPS, op0=ALU.mult, op1=ALU.add), [b_bank[1]], [b_tmpT])
                P.op("act", lambda e: e.activation(out=o2, in_=o2, func=AF.Sqrt), [b_tmpT], [b_tmpT])
                P.op("dve", lambda e: e.reciprocal(out=o2, in_=o2), [b_tmpT], [b_tmpT])
                P.op("dve", lambda e: e.tensor_tensor(out=o1, in0=o1, in1=o2, op=ALU.mult), [b_tmpT], [b_tmpT])
                P.op("act", lambda e, hp=hp: e.activation(out=o1, in_=o1, func=AF.Identity, scale=pcol[:, hp * 5 + 3:hp * 5 + 4], bias=pcol[:, hp * 5 + 4:hp * 5 + 5]),
                     [b_tmpT, b_pcol], [b_tmpT])
                P.op("dve", lambda e, tc_=tc_, hp=hp: e.scalar_tensor_tensor(out=o2, in0=rS[:, tc_], scalar=pcol[:, hp * 5 + 2:hp * 5 + 3], in1=ksum[:, tc_],
                                                                           op0=ALU.mult, op1=ALU.mult), [b_rS, b_ksum, b_pcol], [b_tmpT])
                P.op("pe", lambda e: e.matmul(pbk[0][:], lhsT=bones, rhs=o2, start=True, stop=True), [b_tmpT, b_cst], [b_bank[0]])
                P.op("dve", lambda e, tc_=tc_: e.tensor_tensor(out=o3, in0=pbk[0][:], in1=vS[:, tc_], op=ALU.mult), [b_bank[0], b_vS], [b_tmpT])
                P.op("dve", lambda e: e.tensor_tensor(out=o1, in0=o1, in1=o3, op=ALU.add), [b_tmpT], [b_tmpT])
                for kc in range(2):
                    P.op("pe", lambda e, kc=kc, xc=xc, hp=hp: e.matmul(pbk[1][:], lhsT=gup_bf[:, kc * 256 + hp * 128:kc * 256 + (hp + 1) * 128], rhs=sgx[:, kc, xc],
                                                                     start=(kc == 0), stop=(kc == 1)), [b_lora, b_sgx], [b_bank[1]])
                ms = pi % 2
                P.op("dve", lambda e, ms=ms: e.tensor_tensor(out=mixst[ms][:], in0=o1, in1=pbk[1][:], op=ALU.mult), [b_tmpT, b_bank[1]], [b_mixst[ms]])
                P.dma("sp", mixo[hp * 128:(hp + 1) * 128, xc], mixst[ms][:], b_mixst[ms], reads=[b_mixst[ms]], writes=[b_mixo])
    try:
        _rwkv_all()
    except StopBuild:
        for _e in ("act", "dve", "pool", "pe"):
            _b = Buf("fin"); _b.w = (P.esem[_e], P.cnt[_e]); P.wait_all("sp", [_b])
        P.close()
        return nc
    prev_ar = [b_rS, b_kS, b_vS, b_kk, b_ksum, b_ysum, b_praw, b_tmpT, b_tw, b_xa, b_sgx] + blk_bufs

    if _os.environ.get("PA_STOP") == "2":
        fin = Buf("fin")
        fin.w = (b_mixst[0].dsem, b_mixst[0].dcnt); P.wait_all("sp", [fin])
        fin.w = (b_mixst[1].dsem, b_mixst[1].dcnt); P.wait_all("sp", [fin])
        P.close()
        return nc
    cosT = af(0, 4096); sinT = af(4096, 4096); b_cs = Buf("cossin")
    raw = af(8192, 4352); b_raw = Buf("raw")
    q_bf = abf(12544, 4096).rearrange("p (k t) -> p k t", t=4096); b_q = Buf("q")
    k_bf = abf(16640, 2176); b_k = Buf("k")
    Vtok = abf(18816, 2176).rearrange("p (b c) -> p b c", c=128); b_V = Buf("V")
    ao = abf(20992, 4096).rearrange("p (k t) -> p k t", t=4096); b_ao = Buf("ao")
    t1 = af(25088, 512); t2 = af(25600, 512); b_t12 = Buf("t12")
    Pt = [abf(26112 + i * 320, 320).rearrange("p (k q) -> p k q", q=128) for i in range(2)]; b_Pt = [Buf("Pt0"), Buf("Pt1")]
    mk_bf = abf(26752, 128).rearrange("p (k q) -> p k q", q=128); b_mk = Buf("mk")
    onesb = abf(26880, 64); dn = [af(26944 + i * 128, 128) for i in range(2)]; b_dn = [Buf("dn0"), Buf("dn1")]
    P.dma("sp", cosT, cos_d[:, :], b_cs, writes=[b_cs] + prev_ar)
    P.dma("act", sinT, sin_d[:, :], b_cs, writes=[b_cs] + prev_ar)
    P.op("pool", lambda e: e.memset(onesb, 1.0), [], [b_mk] + prev_ar)
    P.op("dve", lambda e: e.tensor_copy(out=mk_bf[:, 0, :], in_=LI), [b_cst], [b_mk])
    P.op("dve", lambda e: e.tensor_copy(out=mk_bf[:, 1, :], in_=UI), [b_cst], [b_mk])

    def rope(dst_of_piece, b_dst):
        for pi in range(8):
            xc = slice(pi * 512, (pi + 1) * 512)
            rc = slice(256 + pi * 512, 256 + (pi + 1) * 512)
            pk = pi % 2
            P.op("pe", lambda e, rc=rc, pk=pk: e.matmul(pbk[pk][:], lhsT=ropeP, rhs=raw[:, rc], start=True, stop=True), [b_raw, b_cst], [b_bank[pk]])
            P.op("dve", lambda e, xc=xc, pk=pk: e.tensor_tensor(out=t1, in0=pbk[pk][:], in1=sinT[:, xc], op=ALU.mult), [b_bank[pk], b_cs], [b_t12] + prev_ar)
            P.op("pool", lambda e, xc=xc, rc=rc: e.tensor_tensor(out=t2, in0=raw[:, rc], in1=cosT[:, xc], op=ALU.mult), [b_raw, b_cs], [b_t12])
            P.op("dve", lambda e, pi=pi: e.tensor_tensor(out=dst_of_piece(pi), in0=t1, in1=t2, op=ALU.add), [b_t12], [b_dst] + prev_ar)

    for qc in range(2):
        P.dma("sp", raw[:, 256:T], pxT[(10 + qc) * 128:(11 + qc) * 128, 256:T], b_raw, reads=[b_px[10 + qc]], writes=[b_raw] + prev_ar)
        rope(lambda pi, qc=qc: q_bf[:, qc, pi * 512:(pi + 1) * 512], b_q)
    P.dma("sp", raw, pxT[12 * 128:13 * 128, :], b_raw, reads=[b_px[12]], writes=[b_raw])
    P.op("act", lambda e: e.activation(out=k_bf[:, 0:256], in_=raw[:, 0:256], func=AF.Identity), [b_raw], [b_k] + prev_ar)
    rope(lambda pi: k_bf[:, 256 + pi * 512:256 + (pi + 1) * 512], b_k)
    P.dma("sp", raw, pxT[13 * 128:14 * 128, :], b_raw, reads=[b_px[13]], writes=[b_raw])
    for blk in range(34):
        sl, bsl = slots[8 + blk % 4], b_slots[8 + blk % 4]
        P.op("pe", lambda e, blk=blk, sl=sl: e.transpose(out=sl, in_=raw[:, blk * 128:(blk + 1) * 128], identity=ident), [b_raw, b_cst], [bsl])
        P.op("act", lambda e, blk=blk, sl=sl: e.activation(out=Vtok[:, blk, :], in_=sl, func=AF.Identity), [bsl], [b_V] + prev_ar)
    un = 0
    for h in range(4):
        hs = slice((h % 2) * 64, (h % 2 + 1) * 64)
        qc = h // 2
        for n in range(32):
            s = un % 2
            un += 1
            kbs = [(0, None), (1, None)]
            if n > 0:
                kbs.append((2 + n - 1, 0))
            kbs.append((2 + n, None))
            if n < 31:
                kbs.append((2 + n + 1, 1))
            psb = [slots[s * 16 + i] for i in range(5)]; bpsb = [b_slots[s * 16 + i] for i in range(5)]
            for i, (kb, mk) in enumerate(kbs):
                P.op("pe", lambda e, i=i, kb=kb, hs=hs, n=n, qc=qc: e.matmul(psb[i], lhsT=k_bf[hs, kb * 128:(kb + 1) * 128], rhs=q_bf[hs, qc, n * 128:(n + 1) * 128],
                                                                          start=True, stop=True), [b_k, b_q], [bpsb[i]])
                P.op("act", lambda e, i=i, s=s: e.activation(out=Pt[s][:, i, :], in_=psb[i], func=AF.Exp, scale=0.125), [bpsb[i]], [b_Pt[s]] + prev_ar)
                if mk is not None:
                    P.op("pool", lambda e, i=i, s=s, mk=mk: e.tensor_tensor(out=Pt[s][:, i, :], in0=Pt[s][:, i, :], in1=mk_bf[:, mk, :], op=ALU.mult),
                         [b_Pt[s], b_mk], [b_Pt[s]])
            po, bpo = slots[s * 16 + 5], b_slots[s * 16 + 5]
            pd, bpd = slots[s * 16 + 6], b_slots[s * 16 + 6]
            nk = len(kbs)
            for i, (kb, mk) in enumerate(kbs):
                P.op("pe", lambda e, i=i, kb=kb, s=s: e.matmul(po, lhsT=Vtok[:, kb, :], rhs=Pt[s][:, i, :], start=(i == 0), stop=(i == nk - 1)), [b_V, b_Pt[s]], [bpo])
            for i, (kb, mk) in enumerate(kbs):
                P.op("pe", lambda e, i=i, s=s: e.matmul(pd, lhsT=onesb, rhs=Pt[s][:, i, :], start=(i == 0), stop=(i == nk - 1)), [b_mk, b_Pt[s]], [bpd])
            P.op("dve", lambda e, s=s, h=h, hs=hs: e.tensor_scalar(out=dn[s][hs, :], in0=pd[hs, :], scalar1=pcol[hs, 24 + h:25 + h], scalar2=None, op0=ALU.add),
                 [bpd, b_pcol], [b_dn[s]] + prev_ar)
            P.op("dve", lambda e, s=s, hs=hs: e.reciprocal(out=dn[s][hs, :], in_=dn[s][hs, :]), [b_dn[s]], [b_dn[s]])
            P.op("dve", lambda e, s=s, hs=hs, qc=qc, n=n: e.tensor_tensor(out=ao[hs, qc, n * 128:(n + 1) * 128], in0=po[hs, :], in1=dn[s][hs, :], op=ALU.mult),
                 [bpo, b_dn[s]], [b_ao] + prev_ar)
    for qc in range(2):
        P.dma("sp", mixo[256 + qc * 128:256 + (qc + 1) * 128, :], ao[:, qc, :], b_ao, reads=[b_ao], writes=[b_mixo])
    P.wait_all("sp", [b_mixo])
    for ms in range(2):
        P.wait_all("sp", [Buf("dummy")])
    fin = Buf("fin")
    fin.w = (b_mixst[0].dsem, b_mixst[0].dcnt); P.wait_all("sp", [fin])
    fin.w = (b_mixst[1].dsem, b_mixst[1].dcnt); P.wait_all("sp", [fin])
    fin.w = (b_ao.dsem, b_ao.dcnt); P.wait_all("sp", [fin])
    P.close()
    return nc


def rope_tables():
    t = np.arange(4096)
    row = (t // 64).astype(np.float32); col = (t % 64).astype(np.float32)
    freqs = (10000.0 ** (-np.arange(16, dtype=np.float32) / 16)).astype(np.float32)
    cos = np.zeros((64, 4096), np.float32); sin = np.zeros((64, 4096), np.float32)
    for dd in range(64):
        pos = row if dd < 32 else col
        ang = (pos * freqs[dd % 16]).astype(np.float32)
        cos[dd] = np.cos(ang); sin[dd] = np.sin(ang)
    Pm = np.zeros((64, 64), np.float32)
    for base in (0, 32):
        for i in range(16):
            Pm[base + i + 16, base + i] = -1.0
            Pm[base + i, base + i + 16] = 1.0
    P2 = np.zeros((128, 128), np.float32); P2[:64, :64] = Pm; P2[64:, 64:] = Pm
    return np.concatenate([cos, cos], 0), np.concatenate([sin, sin], 0), P2


def run_phaseA(inp):
    nc = build_phaseA()
    w_mod = inp["w_mod"][0]; b_mod = inp["b_mod"][0]; w_in = inp["w_in"][0]
    cos, sin, P2 = rope_tables()
    ii = np.arange(128)
    SL = (ii[None, :] < ii[:, None]).astype(np.float32)
    SU = (ii[None, :] > ii[:, None]).astype(np.float32)
    LI = (ii[None, :] <= ii[:, None]).astype(np.float32)
    UI = (ii[None, :] >= ii[:, None]).astype(np.float32)
    bo = np.zeros((128, 128), np.float32); bo[:64, :64] = 1; bo[64:, 64:] = 1
    cst = np.concatenate([np.eye(128, dtype=np.float32), SL, SU, LI, UI, bo, P2, np.ones((128, 128), np.float32)], axis=1)
    RD = 1024
    o_r, o_k, o_v, o_xw, o_xa, o_xg = 0, RD, 2 * RD, 3 * RD, 3 * RD + 96, 3 * RD + 192
    RC = 3 * RD + 96 + 96 + 256
    o_q, o_ka, o_va = RC, RC + 1024, RC + 1024 + 256
    shared = dict(wmodA=np.ascontiguousarray(w_mod[:, 0:2 * D]),
                  bmodA=np.concatenate([_col(b_mod[0:D], 16), _col(b_mod[D:2 * D], 16)], axis=1),
                  g1col=_col(inp["norm1_g"][0], 16), cst=cst, cos=cos, sin=sin)
    z32 = np.zeros((D, 32), np.float32)
    smu = inp["shift_mu"][0]
    in_maps = []
    for core in range(8):
        b, j = core // 4, core % 4
        hc = slice(256 * j, 256 * (j + 1))
        kv = slice(64 * j, 64 * (j + 1))
        cols = [w_in[:, o_r + 256 * j:o_r + 256 * (j + 1)], w_in[:, o_k + 256 * j:o_k + 256 * (j + 1)], w_in[:, o_v + 256 * j:o_v + 256 * (j + 1)],
                w_in[:, o_xw:o_xw + 96], z32, w_in[:, o_xa:o_xa + 96], z32, w_in[:, o_xg:o_xg + 256],
                w_in[:, o_q + 256 * j:o_q + 256 * (j + 1)],
                w_in[:, o_ka + 64 * j:o_ka + 64 * (j + 1)], w_in[:, o_ka + 64 * j:o_ka + 64 * (j + 1)],
                w_in[:, o_va + 64 * j:o_va + 64 * (j + 1)], w_in[:, o_va + 64 * j:o_va + 64 * (j + 1)]]
        win = np.ascontiguousarray(np.concatenate(cols, axis=1))
        assert win.shape[1] == NCOL
        z32v = np.zeros(32, np.float32)
        muv = np.concatenate([smu[o_r + 256 * j:o_r + 256 * (j + 1)], smu[o_k + 256 * j:o_k + 256 * (j + 1)], smu[o_v + 256 * j:o_v + 256 * (j + 1)],
                              smu[o_xw:o_xw + 96], z32v, smu[o_xa:o_xa + 96], z32v, smu[o_xg:o_xg + 256]])
        pc = np.zeros((128, 10), np.float32)
        db = np.zeros((128, 4), np.float32); ib = np.zeros((128, 4), np.float32)
        for hp in range(2):
            ch = slice(256 * j + 128 * hp, 256 * j + 128 * (hp + 1))
            pc[:, hp * 5 + 0] = inp["k_k"][0][ch]; pc[:, hp * 5 + 1] = inp["k_a"][0][ch]
            pc[:, hp * 5 + 2] = inp["r_k"][0].reshape(-1)[ch]
            pc[:, hp * 5 + 3] = inp["lnx_g"][0][ch]; pc[:, hp * 5 + 4] = inp["lnx_b"][0][ch]
            for dd in range(2):
                db[:, hp * 2 + dd] = inp["decay_bias"][0][dd][ch]; ib[:, hp * 2 + dd] = inp["iclr_bias"][0][dd][ch]
        dup = np.concatenate([inp["decay_up"][0][dd][:, hc] for dd in range(2)], axis=1)
        iup = np.concatenate([inp["iclr_up"][0][dd][:, hc] for dd in range(2)], axis=1)
        gu = inp["gate_up"][0][:, hc]
        gup = np.concatenate([gu[0:128], gu[128:256]], axis=1)
        sink = np.ascontiguousarray(np.broadcast_to(inp["attn_sink"][0][4 * j:4 * j + 4][None, :], (128, 4)))
        xT = np.ascontiguousarray(np.concatenate([inp["ctx"][b], inp["x"][b]], axis=0).T)
        ccm = np.zeros((128, 32), np.float32)
        ccm[:, 0::2] = _col(inp["c"][b], 16); ccm[:, 1::2] = _col(inp["c_ctx"], 16)
        m = dict(shared)
        m.update(xT=xT, cc=ccm, win=win, mu=_col(muv, 10), pcol=pc, dbias=db, ibias=ib, dup=np.ascontiguousarray(dup), iup=np.ascontiguousarray(iup),
                 gup=np.ascontiguousarray(gup), sink=sink)
        in_maps.append(m)
    res = run_bass_kernel_spmd(nc, in_maps, core_ids=list(range(8)))
    mixT = np.zeros((2, D, 4096), dtype=ml_dtypes.bfloat16)
    for core in range(8):
        b, j = core // 4, core % 4
        mo = res.results[core]["mixo"]
        mixT[b, 256 * j:256 * (j + 1), :] = mo[0:256]
        mixT[b, 1024 + 256 * j:1024 + 256 * (j + 1), :] = mo[256:512]
    return mixT


def kernel(**inputs):
    inp = {k: np.asarray(v) for k, v in inputs.items()}
    mixT = run_phaseA(inp)
    return run_phaseB(inp, mixT)


def _col(v, n):
    return np.ascontiguousarray(v.reshape(n, 128).T)


def run_phaseB(inp, mixT_full):
    nc = build_phaseB()
    w_mod = inp["w_mod"][0]; b_mod = inp["b_mod"][0]
    wmodB = np.ascontiguousarray(w_mod[:, 2 * D:6 * D])
    bm = b_mod
    bmod_cols = np.concatenate([_col(bm[3 * D:4 * D], 16), _col(bm[4 * D:5 * D], 16)], axis=1)
    bmod_rows = np.ascontiguousarray(np.broadcast_to(np.concatenate([bm[2 * D:3 * D], bm[5 * D:6 * D]])[None, :], (128, 2 * D)))
    eg = np.concatenate([inp["e_gate"][0], inp["s_gate"]], axis=0)
    eu = np.concatenate([inp["e_up"][0], inp["s_up"]], axis=0)
    ed = np.concatenate([inp["e_down"][0], inp["s_down"]], axis=0)
    shared = dict(wout=inp["w_out"][0], wmodB=wmodB, bmod_cols=bmod_cols, bmod_rows=bmod_rows,
                  g2col=_col(inp["norm2_g"][0], 16), wr=inp["w_router"][0],
                  ebias=np.ascontiguousarray(np.broadcast_to(inp["expert_bias"][0][None, :], (128, 64))),
                  eg=eg, eu=eu, ed=ed,
                  fg=np.ascontiguousarray(np.broadcast_to(inp["final_g"][None, :], (128, D))),
                  ident=np.eye(128, dtype=np.float32))
    in_maps = []
    for core in range(8):
        b, q = core // 4, core % 4
        m = dict(shared)
        m["mixT"] = np.ascontiguousarray(mixT_full[b][:, q * NT_B:(q + 1) * NT_B])
        m["xtok"] = np.ascontiguousarray(inp["x"][b, q * NT_B:(q + 1) * NT_B, :])
        m["ccol"] = _col(inp["c"][b], 16)
        in_maps.append(m)
    res = run_bass_kernel_spmd(nc, in_maps, core_ids=list(range(8)))
    out = np.zeros((2, 4096, D), np.float32)
    for core in range(8):
        b, q = core // 4, core % 4
        out[b, q * NT_B:(q + 1) * NT_B, :] = res.results[core]["out"]
    return out
```
